# Optimizing a Trainium2 kernel written in Bass

```python
import jax
import jax.numpy as jnp
from jax import lax
import numpy as np

D_MODEL = 2048
BATCH = 4
SEQ = 4096
DEPTH = 1

HEAD_DIM = 128
ATT_GROUPS = ((128, 1), (512, 4), (2048, 16))
ATT_HEADS_PER_GROUP = 4
ATT_HEADS = ATT_HEADS_PER_GROUP * len(ATT_GROUPS)
ATT_WIDTH = ATT_HEADS * HEAD_DIM
ATT_OUT_WIDTH = ATT_HEADS_PER_GROUP * HEAD_DIM
ATT_BLOCK = 128
HGRN_HEADS = 8
HGRN_DK = 128
HGRN_DV = 128
HGRN_WIDTH = HGRN_HEADS * HGRN_DK
HGRN_CHUNK = 64
N_EXPERTS = 64
N_GROUPS = 8
TOPK_GROUPS = 4
TOP_K = 8
D_EXPERT = 512
D_SHARED = 512
ROUTED_SCALE = 2.5
MOE_BLOCK = 128
NORM_EPS = 1e-6
IN_COLS = 3 * ATT_WIDTH + 4 * HGRN_WIDTH + 2 * D_MODEL

kernel_name = 'hybrid_dilated_attn_hgrn2_moe_block'


def rmsnorm(a, gain):
    af = a.astype(jnp.float32)
    y = af * lax.rsqrt(jnp.mean(af * af, axis=-1, keepdims=True) + NORM_EPS)
    return (y * gain.astype(jnp.float32)).astype(a.dtype)


def modulate(a, shift, scale):
    return a * (1.0 + scale[:, None, :]) + shift[:, None, :]


def alibi_slopes():
    h = jnp.arange(1, ATT_HEADS + 1, dtype=jnp.float32)
    return jnp.exp2(-8.0 * h / ATT_HEADS)


def dilated_window_attention(q, k, v, window, dilation, slopes):
    B, H, S, hd = q.shape
    w_sub = window // dilation
    L = S // dilation
    nb = -(-L // ATT_BLOCK)
    Lp = nb * ATT_BLOCK

    def to_blocks(a):
        a = a.reshape(B, H, L, dilation, hd).transpose(0, 1, 3, 2, 4)
        a = jnp.pad(a, ((0, 0), (0, 0), (0, 0), (0, Lp - L), (0, 0)))
        return a.reshape(B, H, dilation, nb, ATT_BLOCK, hd)

    qb, kb, vb = to_blocks(q), to_blocks(k), to_blocks(v)

    def with_prev(a):
        prev = jnp.pad(a[:, :, :, :-1], ((0, 0), (0, 0), (0, 0), (1, 0), (0, 0), (0, 0)))
        return jnp.concatenate([prev, a], axis=4)

    kk, vv = with_prev(kb), with_prev(vb)
    s = jnp.einsum('bhrnqd,bhrnkd->bhrnqk', qb, kk, preferred_element_type=jnp.float32) * (hd ** -0.5)
    qi = jnp.arange(ATT_BLOCK)[:, None]
    ki = jnp.arange(2 * ATT_BLOCK)[None, :]
    j = qi + ATT_BLOCK - ki
    key_pos = jnp.arange(nb)[:, None, None] * ATT_BLOCK + ki[None] - ATT_BLOCK
    valid = (j >= 0) & (j <= w_sub) & (key_pos >= 0)
    dist = (dilation * j).astype(jnp.float32)
    s = s - slopes.astype(jnp.float32)[None, :, None, None, None, None] * dist
    s = jnp.where(valid, s, -jnp.inf)
    lse = jax.nn.logsumexp(s, axis=-1)
    p = jnp.exp(s - lse[..., None])
    o = jnp.einsum('bhrnqk,bhrnkd->bhrnqd', p, vv.astype(jnp.float32))
    o = o.reshape(B, H, dilation, Lp, hd)[:, :, :, :L].transpose(0, 1, 3, 2, 4).reshape(B, H, S, hd)
    lse = lse.reshape(B, H, dilation, Lp)[:, :, :, :L].transpose(0, 1, 3, 2).reshape(B, H, S)
    return o, lse


def hgrn2_chunkwise(q, k, v, log_f):
    B, H, S, dk = q.shape
    dv = v.shape[-1]
    n_chunks = S // HGRN_CHUNK

    def to_chunks(a):
        return a.reshape(B, H, n_chunks, HGRN_CHUNK, a.shape[-1]).transpose(2, 0, 1, 3, 4)

    causal = jnp.tril(jnp.ones((HGRN_CHUNK, HGRN_CHUNK), dtype=bool))[:, :, None]

    def step(state, inp):
        qc, kc, vc, gc = inp
        b = jnp.cumsum(gc, axis=2)
        o_inter = jnp.einsum('bhtk,bhkv->bhtv', qc * jnp.exp(b), state)
        rel = b[:, :, :, None, :] - b[:, :, None, :, :]
        decay = jnp.exp(jnp.where(causal, rel, -jnp.inf))
        scores = jnp.einsum('bhtk,bhsk,bhtsk->bhts', qc, kc, decay)
        o_intra = jnp.einsum('bhts,bhsv->bhtv', scores, vc)
        b_last = b[:, :, -1:, :]
        state = (jnp.exp(b_last[:, :, 0, :])[..., None] * state
                 + jnp.einsum('bhsk,bhsv->bhkv', kc * jnp.exp(b_last - b), vc))
        return state, o_inter + o_intra

    state0 = jnp.zeros((B, H, dk, dv), jnp.float32)
    _, o = lax.scan(step, state0, (to_chunks(q), to_chunks(k), to_chunks(v), to_chunks(log_f)))
    return o.transpose(1, 2, 0, 3, 4).reshape(B, H, S, dv)


def moe_ffn(h, w_router, router_bias, w_gate_e, w_up_e, w_down_e, w_gate_s, w_up_s, w_down_s):
    B, S, D = h.shape
    T = B * S
    xt = h.reshape(T, D)
    scores = jax.nn.sigmoid((xt @ w_router).astype(jnp.float32))
    choice = scores + router_bias.astype(jnp.float32)
    per_group = N_EXPERTS // N_GROUPS
    group_score = lax.top_k(choice.reshape(T, N_GROUPS, per_group), 2)[0].sum(-1)
    _, top_groups = lax.top_k(group_score, TOPK_GROUPS)
    group_mask = jnp.any(top_groups[:, :, None] == jnp.arange(N_GROUPS)[None, None, :], axis=1)
    expert_mask = jnp.repeat(group_mask, per_group, axis=1)
    _, top_idx = lax.top_k(jnp.where(expert_mask, choice, -jnp.inf), TOP_K)
    gate = jnp.take_along_axis(scores, top_idx, axis=1)
    gate = gate / jnp.sum(gate, axis=1, keepdims=True) * ROUTED_SCALE

    n_assign = T * TOP_K
    n_blocks = -(-n_assign // MOE_BLOCK) + N_EXPERTS
    flat_e = top_idx.reshape(-1)
    flat_tok = jnp.repeat(jnp.arange(T, dtype=jnp.int32), TOP_K)
    flat_w = gate.reshape(-1)
    order = jnp.argsort(flat_e)
    e_sorted = flat_e[order]
    counts = jax.ops.segment_sum(jnp.ones_like(flat_e), flat_e, num_segments=N_EXPERTS)
    starts = jnp.cumsum(counts) - counts
    padded = (counts + MOE_BLOCK - 1) // MOE_BLOCK * MOE_BLOCK
    pad_end = jnp.cumsum(padded)
    dest = (pad_end - padded)[e_sorted] + jnp.arange(n_assign, dtype=jnp.int32) - starts[e_sorted]
    slot_tok = jnp.full((n_blocks * MOE_BLOCK,), T, jnp.int32).at[dest].set(flat_tok[order])
    slot_w = jnp.zeros((n_blocks * MOE_BLOCK,), jnp.float32).at[dest].set(flat_w[order])
    block_start = jnp.arange(n_blocks, dtype=jnp.int32) * MOE_BLOCK
    block_expert = jnp.minimum(jnp.searchsorted(pad_end, block_start, side='right'), N_EXPERTS - 1)
    x_pad = jnp.concatenate([xt, jnp.zeros((1, D), xt.dtype)], axis=0)

    def expert_block(acc, blk):
        tok, wts, e = blk
        xb = x_pad[tok]
        hid = jax.nn.silu(xb @ w_gate_e[e]) * (xb @ w_up_e[e])
        yb = (hid @ w_down_e[e]).astype(jnp.float32) * wts[:, None]
        return acc.at[tok].add(yb), None

    acc, _ = lax.scan(expert_block, jnp.zeros((T + 1, D), jnp.float32),
                      (slot_tok.reshape(n_blocks, MOE_BLOCK), slot_w.reshape(n_blocks, MOE_BLOCK), block_expert))
    shared = (jax.nn.silu(xt @ w_gate_s) * (xt @ w_up_s)) @ w_down_s
    return (acc[:T] + shared.astype(jnp.float32)).astype(h.dtype).reshape(B, S, D)


def hybrid_layer(x, c, lb, w_ada, b_ada, pre_norm_mix, post_norm_mix, w_in, hgrn_norm,
                 w_branch_attn, w_branch_hgrn, w_out, pre_norm_ffn, post_norm_ffn,
                 w_router, router_bias, w_gate_e, w_up_e, w_down_e, w_gate_s, w_up_s, w_down_s):
    B, S, D = x.shape
    mod = jax.nn.silu(c) @ w_ada + b_ada
    shift_m, scale_m, gate_m, shift_f, scale_f, gate_f = jnp.split(mod, 6, axis=-1)

    h = modulate(rmsnorm(x, pre_norm_mix), shift_m, scale_m)
    proj = h @ w_in
    cuts = [int(v) for v in np.cumsum([ATT_WIDTH] * 3 + [HGRN_WIDTH] * 4 + [D_MODEL])]
    q_a, k_a, v_a, q_r, f_r, i_r, g_r, gate_a, gate_b = jnp.split(proj, cuts, axis=-1)

    def att_heads(a):
        return a.reshape(B, S, ATT_HEADS, HEAD_DIM).transpose(0, 2, 1, 3)
    qh, kh, vh = att_heads(q_a), att_heads(k_a), att_heads(v_a)
    slopes = alibi_slopes()
    outs, lses = [], []
    for g, (window, dilation) in enumerate(ATT_GROUPS):
        sl = slice(g * ATT_HEADS_PER_GROUP, (g + 1) * ATT_HEADS_PER_GROUP)
        o_g, lse_g = dilated_window_attention(qh[:, sl], kh[:, sl], vh[:, sl], window, dilation, slopes[sl])
        outs.append(o_g)
        lses.append(lse_g)
    mix_w = jax.nn.softmax(jnp.stack(lses, axis=0), axis=0)
    o_att = jnp.sum(mix_w[..., None] * jnp.stack(outs, axis=0), axis=0)
    o_att = o_att.transpose(0, 2, 1, 3).reshape(B, S, ATT_OUT_WIDTH).astype(x.dtype)

    f = lb + (1.0 - lb) * jax.nn.sigmoid(f_r.astype(jnp.float32))
    def rec_heads(a):
        return a.reshape(B, S, HGRN_HEADS, -1).transpose(0, 2, 1, 3)
    o_rec = hgrn2_chunkwise(rec_heads(q_r.astype(jnp.float32)), rec_heads(1.0 - f),
                            rec_heads(i_r.astype(jnp.float32)), rec_heads(jnp.log(f)))
    o_rec = rmsnorm(o_rec, hgrn_norm.reshape(HGRN_HEADS, 1, HGRN_DV))
    o_rec = o_rec.transpose(0, 2, 1, 3).reshape(B, S, HGRN_HEADS * HGRN_DV).astype(x.dtype) * jax.nn.silu(g_r)

    merged = jax.nn.sigmoid(gate_a) * (o_att @ w_branch_attn) + jax.nn.sigmoid(gate_b) * (o_rec @ w_branch_hgrn)
    y = merged @ w_out
    x = x + gate_m[:, None, :] * rmsnorm(y, post_norm_mix)

    h2 = modulate(rmsnorm(x, pre_norm_ffn), shift_f, scale_f)
    y2 = moe_ffn(h2, w_router, router_bias, w_gate_e, w_up_e, w_down_e, w_gate_s, w_up_s, w_down_s)
    x = x + gate_f[:, None, :] * rmsnorm(y2, post_norm_ffn)
    return x


def setup_inputs(seed: int = 0) -> dict:
    key = jax.random.key(seed)
    ks = jax.random.split(key, 22)
    L, D = DEPTH, D_MODEL

    def nrm(k, shape, scale):
        return jax.random.normal(k, shape, jnp.float32) * scale

    return {
        'x': nrm(ks[0], (BATCH, SEQ, D), 1.0),
        'c': nrm(ks[1], (BATCH, D), 1.0),
        'w_ada': nrm(ks[2], (L, D, 6 * D), 0.5 * D ** -0.5),
        'b_ada': nrm(ks[3], (L, 6 * D), 0.02),
        'pre_norm_mix': 1.0 + nrm(ks[4], (L, D), 0.1),
        'post_norm_mix': 1.0 + nrm(ks[5], (L, D), 0.1),
        'w_in': nrm(ks[6], (L, D, IN_COLS), D ** -0.5),
        'hgrn_lb_logits': nrm(ks[7], (DEPTH + 1, HGRN_WIDTH), 0.5),
        'hgrn_norm': 1.0 + nrm(ks[8], (L, HGRN_HEADS * HGRN_DV), 0.1),
        'w_branch_attn': nrm(ks[9], (L, ATT_OUT_WIDTH, D), ATT_OUT_WIDTH ** -0.5),
        'w_branch_hgrn': nrm(ks[10], (L, HGRN_HEADS * HGRN_DV, D), (HGRN_HEADS * HGRN_DV) ** -0.5),
        'w_out': nrm(ks[11], (L, D, D), D ** -0.5),
        'pre_norm_ffn': 1.0 + nrm(ks[12], (L, D), 0.1),
        'post_norm_ffn': 1.0 + nrm(ks[13], (L, D), 0.1),
        'w_router': nrm(ks[14], (L, D, N_EXPERTS), D ** -0.5),
        'router_bias': nrm(ks[15], (L, N_EXPERTS), 0.01),
        'w_gate_e': nrm(ks[16], (L, N_EXPERTS, D, D_EXPERT), D ** -0.5),
        'w_up_e': nrm(ks[17], (L, N_EXPERTS, D, D_EXPERT), D ** -0.5),
        'w_down_e': nrm(ks[18], (L, N_EXPERTS, D_EXPERT, D), D_EXPERT ** -0.5),
        'w_gate_s': nrm(ks[19], (L, D, D_SHARED), D ** -0.5),
        'w_up_s': nrm(ks[20], (L, D, D_SHARED), D ** -0.5),
        'w_down_s': nrm(ks[21], (L, D_SHARED, D), D_SHARED ** -0.5),
    }


def reference(x, c, w_ada, b_ada, pre_norm_mix, post_norm_mix, w_in, hgrn_lb_logits, hgrn_norm,
              w_branch_attn, w_branch_hgrn, w_out, pre_norm_ffn, post_norm_ffn, w_router, router_bias,
              w_gate_e, w_up_e, w_down_e, w_gate_s, w_up_s, w_down_s):
    lb_table = jnp.cumsum(jax.nn.softmax(hgrn_lb_logits.astype(jnp.float32), axis=0), axis=0)
    for l in range(DEPTH):
        x = hybrid_layer(x, c, lb_table[l], w_ada[l], b_ada[l], pre_norm_mix[l], post_norm_mix[l], w_in[l],
                         hgrn_norm[l], w_branch_attn[l], w_branch_hgrn[l], w_out[l], pre_norm_ffn[l],
                         post_norm_ffn[l], w_router[l], router_bias[l], w_gate_e[l], w_up_e[l], w_down_e[l],
                         w_gate_s[l], w_up_s[l], w_down_s[l])
    return x
```

```python
import os
import numpy as np
from contextlib import ExitStack
import concourse.bass as bass
import concourse.mybir as mybir
from concourse.bass_utils import run_bass_kernel_spmd

F32 = mybir.dt.float32
BF16 = mybir.dt.bfloat16
AF = mybir.ActivationFunctionType
ALU = mybir.AluOpType
AX = mybir.AxisListType

D = 2048
TOK = 2048
EPS = 1e-6
DIL = (1, 4, 16)
NE = 64
COL_QA, COL_KA, COL_VA = 0, 1536, 3072
COL_QR, COL_FR, COL_IR, COL_GR = 4608, 5632, 6656, 7680
COL_GA, COL_GB = 8704, 10752


class Buf:
    __slots__ = ("name", "w", "r")

    def __init__(self, name):
        self.name = name
        self.w = None
        self.r = {}


class Sched:
    ND = 8

    def __init__(self, nc, es):
        self.nc = nc
        self.engs = {"pe": nc.tensor, "act": nc.scalar, "dve": nc.vector, "pool": nc.gpsimd, "sp": nc.sync}
        self.sems = {}
        self.ccnt = {}
        for e in ("pe", "act", "dve", "pool"):
            self.sems["c_" + e] = es.enter_context(nc.semaphore("c_" + e))
            self.ccnt[e] = 0
        self.dcnt = {}
        self.drr = {}
        for q in ("sp", "pool"):
            for i in range(self.ND):
                self.sems[f"d_{q}{i}"] = es.enter_context(nc.semaphore(f"d_{q}{i}"))
                self.dcnt[(q, i)] = 0
            self.drr[q] = 0
        self.waited = {}

    def _wait(self, eng, t):
        if t is None:
            return
        sid, val = t
        if eng == "pe" and sid == "c_pe":
            return
        key = (eng, sid)
        if self.waited.get(key, 0) >= val:
            return
        self.engs[eng].wait_ge(self.sems[sid], val)
        self.waited[key] = val

    def deps(self, eng, reads, writes):
        for b in reads:
            self._wait(eng, b.w)
        for b in writes:
            self._wait(eng, b.w)
            for sid, val in b.r.items():
                self._wait(eng, (sid, val))

    def commit(self, t, reads, writes):
        for b in reads:
            if b.r.get(t[0], 0) < t[1]:
                b.r[t[0]] = t[1]
        for b in writes:
            b.w = t
            b.r = {}

    def op(self, eng, fn, reads=(), writes=()):
        self.deps(eng, reads, writes)
        ins = fn(self.engs[eng])
        self.ccnt[eng] += 1
        ins.then_inc(self.sems["c_" + eng], 1)
        self.commit(("c_" + eng, self.ccnt[eng]), reads, writes)

    def mm(self, out, pairs, reads, writes, start=True, stop=True, ticket=True, **kw):
        self.deps("pe", reads, writes)
        n = len(pairs)
        ins = None
        for i, (l, r) in enumerate(pairs):
            ins = self.nc.tensor.matmul(out, l, r, start=(start and i == 0), stop=(stop and i == n - 1), **kw)
        if ticket:
            self.ccnt["pe"] += 1
            ins.then_inc(self.sems["c_pe"], 1)
            self.commit(("c_pe", self.ccnt["pe"]), reads, writes)

    def pe_ticket_after(self, ins, reads, writes):
        self.ccnt["pe"] += 1
        ins.then_inc(self.sems["c_pe"], 1)
        self.commit(("c_pe", self.ccnt["pe"]), reads, writes)

    def dma(self, q, out, in_, reads, writes):
        i = self.drr[q]
        self.drr[q] = (i + 1) % self.ND
        sid = f"d_{q}{i}"
        prev = self.dcnt[(q, i)]
        if prev > 0:
            self._wait(q, (sid, 16 * prev))
        self.deps(q, reads, writes)
        self.engs[q].dma_start(out=out, in_=in_).then_inc(self.sems[sid], 16)
        self.dcnt[(q, i)] += 1
        self.commit((sid, 16 * self.dcnt[(q, i)]), reads, writes)

    def barrier(self):
        tickets = [("c_" + e, self.ccnt[e]) for e in ("pe", "act", "dve", "pool") if self.ccnt[e] > 0]
        for (q, i), c in self.dcnt.items():
            if c > 0:
                tickets.append((f"d_{q}{i}", 16 * c))
        for e in ("pe", "act", "dve", "pool", "sp"):
            for t in tickets:
                self._wait(e, t)


def build(stage=99, debug=False):
    nc = bass.Bass("TRN2", target_bir_lowering=False)

    def din(name, shape, dt=F32):
        return nc.dram_tensor(name, list(shape), dt, kind="ExternalInput").ap()

    x_own = din("x_own", [TOK, D])
    x_halo = din("x_halo", [TOK, D])
    flag_d = din("flag", [128, 1])
    cb_d = din("cb", [128, 16])
    w_ada = din("w_ada", [24, 128, 16, 512])
    b_ada = din("b_ada", [1, 6 * D])
    norms_d = din("norms", [4, D])
    w_in = din("w_in", [100, 128, 16, 128])
    lbl_d = din("lbl", [128, 16])
    hn_d = din("hn", [128, 8])
    w_ba = din("w_ba", [16, 128, 4, 128])
    w_bh = din("w_bh", [16, 128, 8, 128])
    w_out = din("w_out", [8, 128, 16, 256])
    w_r = din("w_r", [128, 16, NE])
    rb_d = din("rb", [1, NE])
    NED = NE if stage >= 5 else 1
    w_ge = din("w_ge", [NED, 2, 128, 16, 256])
    w_ue = din("w_ue", [NED, 2, 128, 16, 256])
    w_de = din("w_de", [NED, 128, 4, D])
    w_gs = din("w_gs", [2, 128, 16, 256])
    w_us = din("w_us", [2, 128, 16, 256])
    w_ds = din("w_ds", [128, 4, D])
    ident_d = din("ident", [128, 128])
    tri_d = din("tri32", [128, 128])
    seg_d = din("segmask", [128, 512])
    alibi_d = din("alibi", [12, 128, 256])
    rowm_d = din("rowm", [128, 4])
    out_d = nc.dram_tensor("out", [TOK, D], F32, kind="ExternalOutput").ap()
    modrep = nc.dram_tensor("modrep", [6, 128, D], F32).ap()
    oa_scr = nc.dram_tensor("oa_scr", [128, 12, TOK], BF16).ap()
    h2t_scr = nc.dram_tensor("h2t_scr", [128, 16, TOK], BF16).ap()
    mt_scr = nc.dram_tensor("mt_scr", [16, 128, 16, 128], BF16).ap()
    dbg = {}
    if debug:
        for nm, shp in (("d_h", [128, 16, TOK]), ("d_oa", [128, 12, TOK]), ("d_x1", [TOK, D]), ("d_g", [128, 16, 65])):
            dbg[nm] = nc.dram_tensor(nm, shp, F32, kind="ExternalOutput").ap()

    B_out = [Buf(f"out{i}") for i in range(16)]
    B_modrep = [Buf(f"modrep{i}") for i in range(6)]
    B_oascr = [Buf(f"oascr{i}") for i in range(12)]
    B_h2t = [Buf(f"h2t{i}") for i in range(16)]
    B_mtscr = Buf("mtscr")

    with ExitStack() as es:
        S = Sched(nc, es)

        uid = [0]

        def sb(name, shape, dt, stack=es):
            uid[0] += 1
            name = f"{name}_{uid[0]}"
            t = stack.enter_context(nc.sbuf_tensor(name, list(shape), dt))
            return t, Buf(name)

        ps = []
        psb = []
        for i in range(8):
            ps.append(es.enter_context(nc.psum_tensor(f"ps{i}", [128, 512], F32)))
            psb.append(Buf(f"ps{i}"))

        ident_bf, B_ident = sb("ident_bf", [128, 128], BF16)
        tri, B_tri = sb("tri", [128, 128], F32)
        segm, B_segm = sb("segm", [128, 512], F32)
        ones_bf, B_ones = sb("ones_bf", [128, 128], BF16)
        flag, B_flag = sb("flag_t", [128, 1], F32)
        lbl, B_lbl = sb("lbl_t", [128, 16], F32)
        lb, B_lb = sb("lb", [128, 8], F32)
        oml, B_oml = sb("oml", [128, 8], F32)
        hn, B_hn = sb("hn_t", [128, 8], F32)
        rbias, B_rbias = sb("rbias", [128, NE], F32)
        Gt, B_Gt = sb("Gt", [128, 16, 65], F32)
        S.dma("pool", ident_bf[:], ident_d, [], [B_ident])
        S.dma("sp", tri[:], tri_d, [], [B_tri])
        rowm, B_rowm = sb("rowm", [128, 4], F32)
        S.dma("sp", rowm[:], rowm_d, [], [B_rowm])
        S.dma("sp", segm[:], seg_d, [], [B_segm])
        S.dma("sp", flag[:], flag_d, [], [B_flag])
        S.dma("sp", lbl[:], lbl_d, [], [B_lbl])
        S.dma("sp", hn[:], hn_d, [], [B_hn])
        S.dma("sp", rbias[:], rb_d[0:1, :].to_broadcast([128, NE]), [], [B_rbias])
        S.op("dve", lambda e: e.memset(ones_bf[:], 1.0), [], [B_ones])
        S.op("dve", lambda e: e.memset(Gt[:], 1.0), [], [B_Gt])
        S.op("dve", lambda e: e.tensor_tensor(out=lb[:], in0=lbl[:, 0:8], in1=lbl[:, 8:16], op=ALU.subtract), [B_lbl], [B_lb])
        S.op("act", lambda e: e.activation(out=lb[:], in_=lb[:], func=AF.Sigmoid), [B_lb], [B_lb])
        S.op("dve", lambda e: e.tensor_scalar(out=oml[:], in0=lb[:], scalar1=-1.0, scalar2=1.0, op0=ALU.mult, op1=ALU.add),
             [B_lb], [B_oml])

        NWR = 6
        mid = ExitStack()
        wring = [sb(f"wring{i}", [128, 16, 128], BF16, mid) for i in range(NWR)]
        big, B_hT = sb("hTbig", [128, 16, TOK], BF16, mid)
        hp = ExitStack()
        wr_i = [0]

        def load_wchunk(col0):
            t, b = wring[wr_i[0] % NWR]
            wr_i[0] += 1
            S.dma("pool", t[:], w_in[col0 // 128], [], [b])
            return t, b

        def inproj_fm(w, hT, B_hT, t0, n, pst, B_ps, off=0):
            wt, wb = w
            S.mm(pst[:, off:off + n], [(wt[:, kc, :], hT[:, kc, t0:t0 + n]) for kc in range(16)], [wb, B_hT], [B_ps])

        def inproj_tm(w, hT, B_hT, tsl, pst_ap, B_ps):
            wt, wb = w
            S.mm(pst_ap, [(hT[:, kc, tsl], wt[:, kc, :]) for kc in range(16)], [wb, B_hT], [B_ps])

        def rms_rstd(src_ap, B_src, junk, B_junk, ss, B_ss, nfeat):
            S.op("dve", lambda e: e.memset(ss[:, 0:1], 0.0), [], [B_ss])
            S.op("act", lambda e: e.activation(out=junk, in_=src_ap, func=AF.Square, accum_out=ss[:, 0:1]),
                 [B_src], [B_junk, B_ss])
            S.op("dve", lambda e: e.tensor_scalar(out=ss[:, 1:2], in0=ss[:, 0:1], scalar1=1.0 / nfeat, scalar2=EPS,
                                                  op0=ALU.mult, op1=ALU.add), [B_ss], [B_ss])
            S.op("act", lambda e: e.activation(out=ss[:, 2:3], in_=ss[:, 1:2], func=AF.Sqrt), [B_ss], [B_ss])
            S.op("dve", lambda e: e.reciprocal(out=ss[:, 3:4], in_=ss[:, 2:3]), [B_ss], [B_ss])
            return ss[:, 3:4]

        with ExitStack() as pa:
          if stage >= -1:
                sc, B_sc = sb("sc", [128, 16], F32, pa)
                screp, B_screp = sb("screp", [128, 16, 128], BF16, pa)
                modt, B_modt = sb("modt", [128, D], F32, pa)
                nrm, B_nrm = sb("nrm", [128, D], F32, pa)
                bad, B_bad = sb("bad", [128, D], F32, pa)
                wad = [sb(f"wad{i}", [128, 16, 512], BF16, pa) for i in range(2)]
                S.dma("sp", sc[:], cb_d, [], [B_sc])
                S.op("act", lambda e: e.activation(out=sc[:], in_=sc[:], func=AF.Silu), [B_sc], [B_sc])
                S.op("dve", lambda e: e.tensor_copy(out=screp[:], in_=sc[:].unsqueeze(2).to_broadcast([128, 16, 128])),
                     [B_sc], [B_screp])
                nrm_of = {1: 0, 2: 1, 4: 2, 5: 3}
                for k in range(6):
                    S.dma("sp", bad[:], b_ada[0:1, k * D:(k + 1) * D].to_broadcast([128, D]), [], [B_bad])
                    if k in nrm_of:
                        S.dma("sp", nrm[:], norms_d[nrm_of[k]:nrm_of[k] + 1, :].to_broadcast([128, D]), [], [B_nrm])
                    for n in range(4):
                        wt, wb = wad[(k * 4 + n) % 2]
                        c0 = k * D + n * 512
                        S.dma("pool", wt[:], w_ada[k * 4 + n], [], [wb])
                        pi = (k * 4 + n) % 2
                        S.mm(ps[pi][:], [(screp[:, kc, :], wt[:, kc, :]) for kc in range(16)], [wb, B_screp], [psb[pi]])
                        S.op("dve", lambda e: e.tensor_tensor(out=modt[:, n * 512:(n + 1) * 512], in0=ps[pi][:],
                                                              in1=bad[:, n * 512:(n + 1) * 512], op=ALU.add),
                             [psb[pi], B_bad], [B_modt])
                    if k in (1, 4):
                        S.op("dve", lambda e: e.scalar_tensor_tensor(out=modt[:], in0=modt[:], scalar=1.0, in1=nrm[:],
                                                                     op0=ALU.add, op1=ALU.mult), [B_modt, B_nrm], [B_modt])
                    if k in (2, 5):
                        S.op("dve", lambda e: e.tensor_tensor(out=modt[:], in0=modt[:], in1=nrm[:], op=ALU.mult),
                             [B_modt, B_nrm], [B_modt])
                    S.dma("sp", modrep[k], modt[:], [B_modt], [B_modrep[k]])
                S.barrier()

        def make_hT(xsrc, hT, B_hT, pstk):
            G1, B_G1 = sb("G1", [128, D], F32, pstk)
            SH1, B_SH1 = sb("SH1", [128, D], F32, pstk)
            xr = [sb(f"xin{i}", [128, D], F32, pstk) for i in range(2)]
            hb, B_hb = sb("hb", [128, D], BF16, pstk)
            junk, B_junk = sb("junk", [128, D], BF16, pstk)
            ss, B_ss = sb("ss", [128, 4], F32, pstk)
            S.dma("sp", G1[:], modrep[1], [B_modrep[1]], [B_G1])
            S.dma("sp", SH1[:], modrep[0], [B_modrep[0]], [B_SH1])
            for t in range(16):
                xin, B_x = xr[t % 2]
                S.dma("sp", xin[:], xsrc[t * 128:(t + 1) * 128, :], [], [B_x])
                rstd = rms_rstd(xin[:], B_x, junk[:], B_junk, ss, B_ss, D)
                S.op("dve", lambda e: e.scalar_tensor_tensor(out=xin[:], in0=xin[:], scalar=rstd, in1=G1[:],
                                                             op0=ALU.mult, op1=ALU.mult), [B_x, B_ss, B_G1], [B_x])
                S.op("dve", lambda e: e.tensor_tensor(out=hb[:], in0=xin[:], in1=SH1[:], op=ALU.add), [B_x, B_SH1], [B_hb])
                for half in range(2):
                    pi = 2 + half
                    pv = ps[pi][:].bitcast(BF16).rearrange("p (k t) -> p k t", t=128)
                    S.deps("pe", [B_hb, B_ident], [psb[pi]])
                    ins = None
                    for k in range(8):
                        kc = half * 8 + k
                        ins = nc.tensor.transpose(pv[:, k, :], hb[:, kc * 128:(kc + 1) * 128], ident_bf[:])
                    S.pe_ticket_after(ins, [B_hb, B_ident], [psb[pi]])
                    eng = "act" if half == 0 else "dve"
                    if eng == "act":
                        S.op("act", lambda e: e.copy(out=hT[:, half * 8:half * 8 + 8, t * 128:(t + 1) * 128], in_=pv),
                             [psb[pi]], [B_hT])
                    else:
                        S.op("dve", lambda e: e.tensor_copy(out=hT[:, half * 8:half * 8 + 8, t * 128:(t + 1) * 128], in_=pv),
                             [psb[pi]], [B_hT])

        KH3, B_KH3 = sb("KH3", [128, 4, 2048], BF16, hp)
        VH3, B_VH3 = sb("VH3", [128, 4, 16, 128], BF16, hp)
        KH2, B_KH2 = sb("KH2", [128, 4, 512], BF16, hp)
        VH2, B_VH2 = sb("VH2", [128, 4, 4, 128], BF16, hp)
        KH1, B_KH1 = sb("KH1", [128, 4, 128], BF16, hp)
        VH1, B_VH1 = sb("VH1", [128, 4, 1, 128], BF16, hp)
        SH, B_SH = sb("SHst", [128, 8, 128], F32, hp)
        KH = (KH1, KH2, KH3)
        B_KH = (B_KH1, B_KH2, B_KH3)
        VH = (VH1, VH2, VH3)
        B_VH = (B_VH1, B_VH2, B_VH3)


        def hgrn_front(hh, hT, t0, own, wts, tmp):
            wq, wf, wi, wg = wts
            (f1, B_f1), (f2, B_f2), (f3, B_f3), (f4, B_f4), (f5, B_f5) = tmp["f"]
            (kdT, B_kdT), (kbT, B_kbT), (qbT, B_qbT) = tmp["kq"]
            (kdk, B_kdk), (Vtok, B_Vtok), (eL, B_eL), (AT, B_AT) = tmp["m"]
            inproj_fm(wf, hT, B_hT, t0, 512, ps[0], psb[0])
            if own:
                inproj_fm(wq, hT, B_hT, t0, 512, ps[1], psb[1])
            pv3 = ps[3][:].rearrange("p (k t) -> p k t", t=128)
            for i in range(4):
                S.mm(pv3[:, i, :], [(hT[:, kc, t0 + i * 128:t0 + (i + 1) * 128], wi[0][:, kc, :]) for kc in range(16)],
                     [wi[1], B_hT], [psb[3]])
            if own:
                inproj_fm(wg, hT, B_hT, t0, 512, ps[7], psb[7])
            S.op("act", lambda e: e.activation(out=f1[:], in_=ps[0][:], func=AF.Sigmoid), [psb[0]], [B_f1])
            S.op("dve", lambda e: e.tensor_scalar(out=f2[:], in0=f1[:], scalar1=oml[:, hh:hh + 1], scalar2=lb[:, hh:hh + 1],
                                                  op0=ALU.mult, op1=ALU.add), [B_f1, B_oml, B_lb], [B_f2])
            yield
            S.op("act", lambda e: e.activation(out=f1[:], in_=f2[:], func=AF.Ln), [B_f2], [B_f1])
            S.op("dve", lambda e: e.tensor_tensor_scan(out=f3[:], data0=segm[:], data1=f1[:], initial=0.0,
                                                       op0=ALU.mult, op1=ALU.add), [B_f1, B_segm], [B_f3])
            b3 = f3[:].rearrange("p (c k) -> p c k", k=32)
            S.op("dve", lambda e: e.tensor_tensor(out=f1[:].rearrange("p (c k) -> p c k", k=32),
                                                  in0=b3[:, :, 31:32].to_broadcast([128, 16, 32]), in1=b3, op=ALU.subtract),
                 [B_f3], [B_f1])
            yield
            S.op("act", lambda e: e.activation(out=f4[:], in_=f1[:], func=AF.Exp), [B_f1], [B_f4])
            if own:
                S.op("act", lambda e: e.activation(out=f5[:], in_=f3[:], func=AF.Exp, scale=-1.0), [B_f3], [B_f5])
            S.op("act", lambda e: e.activation(out=eL[:].unsqueeze(2), in_=b3[:, :, 31:32], func=AF.Exp), [B_f3], [B_eL])
            S.op("dve", lambda e: e.tensor_scalar(out=f2[:], in0=f2[:], scalar1=-1.0, scalar2=1.0, op0=ALU.mult, op1=ALU.add),
                 [B_f2], [B_f2])
            S.op("dve", lambda e: e.tensor_tensor(out=kdT[:], in0=f2[:], in1=f4[:], op=ALU.mult), [B_f2, B_f4], [B_kdT])
            S.op("act", lambda e: e.copy(out=Vtok[:], in_=pv3), [psb[3]], [B_Vtok])
            Vm, B_Vm = tmp["vm"]
            for c in range(4):
                S.op("dve", lambda e: e.tensor_scalar(out=Vm[:, c, :, :], in0=Vtok[:], scalar1=rowm[:, c:c + 1], scalar2=None,
                                                      op0=ALU.mult), [B_Vtok, B_rowm], [B_Vm])
            yield
            if own:
                S.op("dve", lambda e: e.tensor_tensor(out=kbT[:], in0=f2[:], in1=f5[:], op=ALU.mult), [B_f2, B_f5], [B_kbT])
                S.op("act", lambda e: e.activation(out=f4[:], in_=f3[:], func=AF.Exp), [B_f3, B_kdT], [B_f4])
                S.op("dve", lambda e: e.tensor_tensor(out=qbT[:], in0=ps[1][:], in1=f4[:], op=ALU.mult), [psb[1], B_f4], [B_qbT])
                S.op("act", lambda e: e.activation(out=f5[:], in_=ps[7][:], func=AF.Silu), [psb[7], B_kbT], [B_f5])

        def hgrn_back(hh, t0, own, tmp, Sst, B_S, Sbf, curbox, ORT=None, B_ORT=None):
            cur = curbox[0]
            (f1, B_f1), (f2, B_f2), (f3, B_f3), (f4, B_f4), (f5, B_f5) = tmp["f"]
            (kdT, B_kdT), (kbT, B_kbT), (qbT, B_qbT) = tmp["kq"]
            (kdk, B_kdk), (Vtok, B_Vtok), (eL, B_eL), (AT, B_AT) = tmp["m"]
            Vm, B_Vm = tmp["vm"]
            pv = ps[2][:].bitcast(BF16)[:, 0:512].rearrange("p (k t) -> p k t", t=128)
            pA = ps[2][:, 256:384]
            S.deps("pe", [B_kdT, B_ident], [psb[2]])
            ins = None
            for i in range(4):
                ins = nc.tensor.transpose(pv[:, i, :], kdT[:, i * 128:(i + 1) * 128], ident_bf[:])
            S.pe_ticket_after(ins, [B_kdT, B_ident], [psb[2]])
            S.op("act", lambda e: e.copy(out=kdk[:], in_=pv), [psb[2]], [B_kdk])
            for i in range(4):
                if i > 0:
                    yield
                tsl = slice(i * 128, (i + 1) * 128)
                pU = ps[5 + (i % 2)]
                B_pU = psb[5 + (i % 2)]
                pUv = pU[:].rearrange("p (k t) -> p k t", t=128)
                for c in range(4):
                    S.mm(pUv[:, c, :], [(kdk[:, i, :], Vm[:, c, i, :])], [B_kdk, B_Vm], [B_pU])
                if own:
                    S.mm(pA, [(kbT[:, tsl], qbT[:, tsl])], [B_kbT, B_qbT], [psb[2]])
                    S.op("dve", lambda e: e.tensor_tensor(out=AT[:], in0=pA, in1=tri[:], op=ALU.mult),
                         [psb[2], B_tri], [B_AT])
                    S.mm(ps[4][:, tsl], [(Vtok[:, i, :], AT[:])], [B_Vtok, B_AT], [psb[4]], stop=False, skip_group_check=True)
                for c in range(4):
                    if own:
                        csl = slice(i * 128 + 32 * c, i * 128 + 32 * c + 32)
                        S.mm(ps[4][:, csl], [(Sbf[cur][0][:], qbT[:, csl])], [Sbf[cur][1], B_qbT], [psb[4]],
                             start=False, stop=True, skip_group_check=True)
                    S.op("dve", lambda e: e.scalar_tensor_tensor(out=Sst, in0=Sst, scalar=eL[:, i * 4 + c:i * 4 + c + 1],
                                                                 in1=pUv[:, c, :], op0=ALU.mult, op1=ALU.add),
                         [B_S, B_eL, B_pU], [B_S])
                    cur = (cur + 1) % len(Sbf)
                    if own:
                        S.op("dve", lambda e: e.tensor_copy(out=Sbf[cur][0][:], in_=Sst), [B_S], [Sbf[cur][1]])
            if own:
                (sq, B_sq), (o1, B_o1) = tmp["o"]
                S.op("act", lambda e: e.activation(out=sq[:], in_=ps[4][:], func=AF.Square), [psb[4]], [B_sq])
                S.mm(ps[0][:], [(ones_bf[:], sq[:])], [B_ones, B_sq], [psb[0]])
                S.op("dve", lambda e: e.tensor_scalar(out=f1[:], in0=ps[0][:], scalar1=1.0 / 128, scalar2=EPS,
                                                      op0=ALU.mult, op1=ALU.add), [psb[0]], [B_f1])
                S.op("act", lambda e: e.activation(out=f1[:], in_=f1[:], func=AF.Sqrt), [B_f1], [B_f1])
                S.op("dve", lambda e: e.reciprocal(out=f1[:], in_=f1[:]), [B_f1], [B_f1])
                S.op("dve", lambda e: e.tensor_tensor(out=o1[:], in0=ps[4][:], in1=f1[:], op=ALU.mult), [psb[4], B_f1], [B_o1])
                S.op("dve", lambda e: e.scalar_tensor_tensor(out=ORT[:, t0:t0 + 512], in0=o1[:], scalar=hn[:, hh:hh + 1],
                                                             in1=f5[:], op0=ALU.mult, op1=ALU.mult),
                     [B_o1, B_hn, B_f5], [B_ORT])
            curbox[0] = cur
            yield

        def hgrn_tmp(stk):
            return {
                "f": [sb(f"hf{i}", [128, 512], F32, stk) for i in range(5)],
                "kq": [sb(f"hk{i}", [128, 512], BF16, stk) for i in range(3)],
                "m": [sb("kdk", [128, 4, 128], BF16, stk), sb("Vtok", [128, 4, 128], BF16, stk),
                      sb("eL", [128, 16], F32, stk), sb("AT", [128, 128], BF16, stk)],
                "o": [sb("sq", [128, 512], BF16, stk), sb("o1", [128, 512], F32, stk)],
                "vm": sb("Vm", [128, 4, 4, 128], BF16, stk),
            }

        with ExitStack() as ph:
          if stage >= 0:
                with ExitStack() as ph0:
                    make_hT(x_halo, big, B_hT, ph0)
                    S.barrier()
                cpi = 0
                HSTOP = int(os.environ.get("HSTOP", "9"))
                for g in (range(3) if HSTOP >= 2 else []):
                    d = DIL[g]
                    span = 128 * d
                    for j in range(4):
                        hh = g * 4 + j
                        wk = load_wchunk(COL_KA + hh * 128)
                        wv = load_wchunk(COL_VA + hh * 128)
                        step = min(512, span)
                        for u0 in range(TOK - span, TOK, step):
                            pi = cpi % 2
                            cpi += 1
                            inproj_fm(wk, big, B_hT, u0, step, ps[pi], psb[pi])
                            o0 = u0 - (TOK - span)
                            S.op("act", lambda e: e.copy(out=KH[g][:, j, o0:o0 + step], in_=ps[pi][:, 0:step]), [psb[pi]], [B_KH[g]])
                        for r0 in range(0, d, 4):
                            nb = min(4, d - r0)
                            pi = 2 + (cpi % 2)
                            cpi += 1
                            pv = ps[pi][:].rearrange("p (k t) -> p k t", t=128)
                            for rr in range(nb):
                                r = r0 + rr
                                tsl = slice(TOK - span + r, TOK, d)
                                S.mm(pv[:, rr, :], [(big[:, kc, tsl], wv[0][:, kc, :]) for kc in range(16)], [wv[1], B_hT], [psb[pi]],
                                     ticket=True)
                            S.op("dve", lambda e: e.tensor_copy(out=VH[g][:, j, r0:r0 + nb, :], in_=pv[:, 0:nb, :]), [psb[pi]], [B_VH[g]])
                with ExitStack() as phh:
                    gfull, B_gf = sb("gfull", [128, TOK], F32, phh)
                    omf, B_omf = sb("omfull", [128, TOK], F32, phh)
                    Bc, B_Bc = sb("Bcum", [128, TOK], F32, phh)
                    kdTf, B_kdTf = sb("kdTf", [128, TOK], BF16, phh)
                    kdtok, B_kdtok = sb("kdtok", [128, 16, 128], BF16, phh)
                    Vth, B_Vth = sb("Vth", [128, 16, 128], BF16, phh)
                    fs = [sb(f"fsig{i}", [128, 512], F32, phh) for i in range(2)]
                    ones5, B_ones5 = sb("ones5", [128, 512], F32, phh)
                    S.op("dve", lambda e: e.memset(ones5[:], 1.0), [], [B_ones5])
                    S.op("dve", lambda e: e.memset(SH[:], 0.0), [], [B_SH])
                    for hh in (range(8) if HSTOP >= 3 else []):
                        wf = load_wchunk(COL_FR + hh * 128)
                        wi = load_wchunk(COL_IR + hh * 128)
                        for seg in range(4):
                            t0 = seg * 512
                            tsl = slice(t0, t0 + 512)
                            pf_, B_pf = ps[seg % 2], psb[seg % 2]
                            f1, B_f1 = fs[seg % 2]
                            inproj_fm(wf, big, B_hT, t0, 512, pf_, B_pf)
                            S.op("act", lambda e: e.activation(out=f1[:], in_=pf_[:], func=AF.Sigmoid), [B_pf], [B_f1])
                            S.op("dve", lambda e: e.tensor_scalar(out=omf[:, tsl], in0=f1[:], scalar1=oml[:, hh:hh + 1],
                                                                  scalar2=lb[:, hh:hh + 1], op0=ALU.mult, op1=ALU.add),
                                 [B_f1, B_oml, B_lb], [B_omf])
                            S.op("act", lambda e: e.activation(out=gfull[:, tsl], in_=omf[:, tsl], func=AF.Ln), [B_omf], [B_gf])
                            S.op("dve", lambda e: e.tensor_scalar(out=omf[:, tsl], in0=omf[:, tsl], scalar1=-1.0, scalar2=1.0,
                                                                  op0=ALU.mult, op1=ALU.add), [B_omf, B_gf], [B_omf])
                            init = 0.0 if seg == 0 else Bc[:, t0 - 1:t0]
                            S.op("dve", lambda e: e.tensor_tensor_scan(out=Bc[:, tsl], data0=ones5[:], data1=gfull[:, tsl], initial=init,
                                                                       op0=ALU.mult, op1=ALU.add), [B_gf, B_ones5, B_Bc], [B_Bc])
                            pvb = ps[2 + seg % 2]
                            B_pvb = psb[2 + seg % 2]
                            pv3 = pvb[:].rearrange("p (k t) -> p k t", t=128)
                            for i in range(4):
                                S.mm(pv3[:, i, :], [(big[:, kc, t0 + i * 128:t0 + (i + 1) * 128], wi[0][:, kc, :]) for kc in range(16)],
                                     [wi[1], B_hT], [B_pvb])
                            if seg % 2 == 0:
                                S.op("act", lambda e: e.copy(out=Vth[:, seg * 4:seg * 4 + 4, :], in_=pv3), [B_pvb], [B_Vth])
                            else:
                                S.op("dve", lambda e: e.tensor_copy(out=Vth[:, seg * 4:seg * 4 + 4, :], in_=pv3), [B_pvb], [B_Vth])
                        S.op("dve", lambda e: e.tensor_tensor(out=gfull[:], in0=Bc[:, TOK - 1:TOK].to_broadcast([128, TOK]), in1=Bc[:],
                                                              op=ALU.subtract), [B_Bc, B_gf], [B_gf])
                        S.op("act", lambda e: e.activation(out=gfull[:], in_=gfull[:], func=AF.Exp), [B_gf], [B_gf])
                        S.op("dve", lambda e: e.tensor_tensor(out=kdTf[:], in0=omf[:], in1=gfull[:], op=ALU.mult), [B_omf, B_gf], [B_kdTf])
                        for half in range(2):
                            pi = 4 + half
                            pv = ps[pi][:].bitcast(BF16).rearrange("p (k t) -> p k t", t=128)
                            S.deps("pe", [B_kdTf, B_ident], [psb[pi]])
                            ins = None
                            for k in range(8):
                                i = half * 8 + k
                                ins = nc.tensor.transpose(pv[:, k, :], kdTf[:, i * 128:(i + 1) * 128], ident_bf[:])
                            S.pe_ticket_after(ins, [B_kdTf, B_ident], [psb[pi]])
                            if half == 0:
                                S.op("act", lambda e: e.copy(out=kdtok[:, 0:8, :], in_=pv), [psb[pi]], [B_kdtok])
                            else:
                                S.op("dve", lambda e: e.tensor_copy(out=kdtok[:, 8:16, :], in_=pv), [psb[pi]], [B_kdtok])
                        S.mm(ps[6][:, 0:128], [(kdtok[:, i, :], Vth[:, i, :]) for i in range(16)], [B_kdtok, B_Vth], [psb[6]])
                        S.op("dve", lambda e: e.tensor_copy(out=SH[:, hh, :], in_=ps[6][:, 0:128]), [psb[6]], [B_SH])
                    S.op("dve", lambda e: e.tensor_scalar(out=SH[:], in0=SH[:], scalar1=flag[:, 0:1], scalar2=None, op0=ALU.mult),
                         [B_SH, B_flag], [B_SH])
                S.barrier()

        with ExitStack() as pb:
            if stage >= 1:
                make_hT(x_own, big, B_hT, pb)
            S.barrier()
        if debug and stage >= 1:
            with ExitStack() as pd:
                dt_, B_dt = sb("dbgt", [128, 16, 512], F32, pd)
                for s4 in range(4):
                    S.op("dve", lambda e: e.tensor_copy(out=dt_[:], in_=big[:, :, s4 * 512:(s4 + 1) * 512]), [B_hT], [B_dt])
                    S.dma("sp", dbg["d_h"][:, :, s4 * 512:(s4 + 1) * 512], dt_[:], [B_dt], [])
                S.barrier()

        if stage >= 2:
            with ExitStack() as pdd:
                QT, B_QT = sb("QT", [128, TOK], BF16, pdd)
                KT, B_KT = sb("KT", [128, TOK], BF16, pdd)
                VB, B_VB = sb("VB", [128, 16, 128], BF16, pdd)
                ND, B_NUM = sb("ND", [128, 2, TOK], F32, pdd)
                B_DEN = B_NUM
                NUM = ND[:, 0, :]
                DEN = ND[:, 1, :]
                OA, B_OA = sb("OA", [128, TOK], BF16, pdd)
                ebt, B_ebt = sb("ebt", [128, 256], F32, pdd)
                ebf, B_ebf = sb("ebf", [128, 256], F32, pdd)
                esb = [sb(f"esb{i}", [128, 256], F32, pdd) for i in range(2)]
                Pt = [sb(f"Pt{i}", [128, 256], BF16, pdd) for i in range(2)]
                cpi = 0
                for j in range(4):
                    for g in range(3):
                        d = DIL[g]
                        span = 128 * d
                        hh = g * 4 + j
                        S.dma("sp", ebt[:], alibi_d[hh], [], [B_ebt])
                        S.op("dve", lambda e: e.tensor_copy(out=ebf[:, 128:256], in_=ebt[:, 128:256]), [B_ebt], [B_ebf])
                        S.op("dve", lambda e: e.tensor_scalar(out=ebf[:, 0:128], in0=ebt[:, 0:128], scalar1=flag[:, 0:1],
                                                              scalar2=None, op0=ALU.mult), [B_ebt, B_flag], [B_ebf])
                        wq = load_wchunk(COL_QA + hh * 128)
                        wk = load_wchunk(COL_KA + hh * 128)
                        wv = load_wchunk(COL_VA + hh * 128)
                        for s4 in range(4):
                            pi = cpi % 2
                            cpi += 1
                            inproj_fm(wq, big, B_hT, s4 * 512, 512, ps[pi], psb[pi])
                            S.op("act", lambda e: e.copy(out=QT[:, s4 * 512:(s4 + 1) * 512], in_=ps[pi][:]), [psb[pi]], [B_QT])
                            pi = cpi % 2
                            cpi += 1
                            inproj_fm(wk, big, B_hT, s4 * 512, 512, ps[pi], psb[pi])
                            S.op("dve", lambda e: e.tensor_copy(out=KT[:, s4 * 512:(s4 + 1) * 512], in_=ps[pi][:]), [psb[pi]], [B_KT])
                        for b0 in range(0, 16, 4):
                            pi = 2 + (cpi % 2)
                            cpi += 1
                            pv = ps[pi][:].rearrange("p (k t) -> p k t", t=128)
                            for bb in range(4):
                                blk = b0 + bb
                                nsp, r = blk // d, blk % d
                                tsl = slice(nsp * span + r, (nsp + 1) * span, d)
                                S.mm(pv[:, bb, :], [(big[:, kc, tsl], wv[0][:, kc, :]) for kc in range(16)], [wv[1], B_hT],
                                     [psb[pi]], ticket=True)
                            S.op("act", lambda e: e.copy(out=VB[:, b0:b0 + 4, :], in_=pv), [psb[pi]], [B_VB])
                        DSTOP = int(os.environ.get("DSTOP", "9"))
                        for blk in (range(16) if DSTOP >= 2 else []):
                            nsp, r = blk // d, blk % d
                            qsl = slice(nsp * span + r, (nsp + 1) * span, d)
                            if nsp == 0:
                                kprev = KH[g][:, j, r:span:d]
                                vprev = VH[g][:, j, r, :]
                                rdk = [B_KH[g]]
                                rdv = [B_VH[g]]
                                tab, B_tab = ebf, B_ebf
                            else:
                                kprev = KT[:, (nsp - 1) * span + r:nsp * span:d]
                                vprev = VB[:, (nsp - 1) * d + r, :]
                                rdk = []
                                rdv = []
                                tab, B_tab = ebt, B_ebt
                            bi = blk % 2
                            pS, B_pS = ps[4 + bi], psb[4 + bi]
                            pO, B_pO = ps[6 + bi], psb[6 + bi]
                            S.mm(pS[:, 0:128], [(kprev, QT[:, qsl])], [B_QT, B_KT] + rdk, [B_pS], ticket=True)
                            S.mm(pS[:, 128:256], [(KT[:, qsl], QT[:, qsl])], [B_QT, B_KT] + rdk, [B_pS])
                            et, B_et = esb[bi]
                            pt, B_pt = Pt[bi]
                            S.op("act", lambda e: e.activation(out=et[:], in_=pS[:, 0:256], func=AF.Exp, scale=128 ** -0.5),
                                 [B_pS], [B_et])
                            S.op("dve", lambda e: e.tensor_tensor(out=pt[:], in0=et[:], in1=tab[:], op=ALU.mult),
                                 [B_et, B_tab], [B_pt])
                            if DSTOP < 3:
                                continue
                            S.mm(pO[:, 0:128], [(vprev, pt[:, 0:128]), (VB[:, blk, :], pt[:, 128:256])], [B_pt, B_VB] + rdv,
                                 [B_pO], ticket=True)
                            S.mm(pO[:, 128:256], [(ones_bf[:], pt[:, 0:128]), (ones_bf[:], pt[:, 128:256])], [B_pt, B_ones], [B_pO])
                            if DSTOP < 4:
                                continue
                            pOv = pO[:, 0:256].rearrange("p (a q) -> p a q", a=2)
                            if g == 0:
                                S.op("dve", lambda e: e.tensor_copy(out=ND[:, :, qsl], in_=pOv), [B_pO], [B_NUM])
                            else:
                                S.op("dve", lambda e: e.tensor_tensor(out=ND[:, :, qsl], in0=ND[:, :, qsl], in1=pOv, op=ALU.add),
                                     [B_pO, B_NUM], [B_NUM])
                    S.op("dve", lambda e: e.reciprocal(out=DEN, in_=DEN), [B_DEN], [B_DEN])
                    S.op("dve", lambda e: e.tensor_tensor(out=OA[:], in0=NUM, in1=DEN, op=ALU.mult), [B_NUM], [B_OA])
                    S.dma("sp", oa_scr[:, j, :], OA[:], [B_OA], [B_oascr[j]])
                S.barrier()

        if stage >= 3:
            with ExitStack() as pe_:
                tmps = [hgrn_tmp(pe_), hgrn_tmp(pe_)]
                Sbf = [sb(f"Sbf{i}", [128, 128], BF16, pe_) for i in range(8)]
                orts = [sb(f"ORT{i}", [128, TOK], BF16, pe_) for i in range(2)]
                units = [(hh, seg) for hh in range(8) for seg in range(4)]
                wts_h = {}
                curbox = [0]

                def drain(g):
                    for _ in g:
                        pass

                for ui in range(len(units) + 1):
                    fr = None
                    bk = None
                    if ui < len(units):
                        hh, seg = units[ui]
                        if seg == 0:
                            wts_h[hh] = (load_wchunk(COL_QR + hh * 128), load_wchunk(COL_FR + hh * 128),
                                         load_wchunk(COL_IR + hh * 128), load_wchunk(COL_GR + hh * 128))
                        fr = hgrn_front(hh, big, seg * 512, True, wts_h[hh], tmps[ui % 2])
                    if ui > 0:
                        ph_, ps_ = units[ui - 1]
                        ORT, B_ORT = orts[ph_ % 2]
                        if ps_ == 0:
                            S.op("act", lambda e: e.copy(out=Sbf[0][0][:], in_=SH[:, ph_, :]), [B_SH], [Sbf[0][1]])
                            curbox[0] = 0
                        bk = hgrn_back(ph_, ps_ * 512, True, tmps[(ui - 1) % 2], SH[:, ph_, :], B_SH, Sbf, curbox, ORT, B_ORT)
                    if fr is not None:
                        drain(fr)
                    if bk is not None:
                        drain(bk)
                    if ui > 0 and ps_ == 3:
                        S.dma("sp", oa_scr[:, 4 + ph_, :], ORT[:], [B_ORT], [B_oascr[4 + ph_]])
                S.barrier()
        if debug and stage >= 2:
            with ExitStack() as pd:
                db_, B_db = sb("dbgb", [128, 12, 512], BF16, pd)
                dt_, B_dt = sb("dbgt", [128, 12, 512], F32, pd)
                for s4 in range(4):
                    S.dma("sp", db_[:], oa_scr[:, :, s4 * 512:(s4 + 1) * 512], B_oascr, [B_db])
                    S.op("dve", lambda e: e.tensor_copy(out=dt_[:], in_=db_[:]), [B_db], [B_dt])
                    S.dma("sp", dbg["d_oa"][:, :, s4 * 512:(s4 + 1) * 512], dt_[:], [B_dt], [])
                S.barrier()

        hp.close()
        if stage >= 4:
            with ExitStack() as pf:
                OAall, B_OAall = sb("OAall", [128, 12, TOK], BF16, pf)
                wbr = [sb(f"wbr{i}", [128, 12, 128], BF16, pf) for i in range(2)]
                sg = [sb(f"sg{i}", [128, 512], F32, pf) for i in range(2)]
                m1, B_m1 = sb("m1", [128, 512], F32, pf)
                m2, B_m2 = sb("m2", [128, 512], F32, pf)
                mtc = [sb(f"mtc{i}", [128, TOK], BF16, pf) for i in range(2)]
                S.dma("sp", OAall[:], oa_scr, B_oascr, [B_OAall])
                for c in range(16):
                    wga = load_wchunk(COL_GA + c * 128)
                    wgb = load_wchunk(COL_GB + c * 128)
                    wb_t, B_wb = wbr[c % 2]
                    S.dma("pool", wb_t[:, 0:4, :], w_ba[c], [], [B_wb])
                    S.dma("pool", wb_t[:, 4:12, :], w_bh[c], [], [B_wb])
                    MTc, B_MTc = mtc[c % 2]
                    for s4 in range(4):
                        t0 = s4 * 512
                        inproj_fm(wga, big, B_hT, t0, 512, ps[0], psb[0])
                        S.op("act", lambda e: e.activation(out=sg[0][0][:], in_=ps[0][:], func=AF.Sigmoid), [psb[0]], [sg[0][1]])
                        S.mm(ps[1][:], [(wb_t[:, jj, :], OAall[:, jj, t0:t0 + 512]) for jj in range(4)], [B_wb, B_OAall], [psb[1]])
                        S.op("dve", lambda e: e.tensor_tensor(out=m1[:], in0=sg[0][0][:], in1=ps[1][:], op=ALU.mult),
                             [sg[0][1], psb[1]], [B_m1])
                        inproj_fm(wgb, big, B_hT, t0, 512, ps[2], psb[2])
                        S.op("act", lambda e: e.activation(out=sg[1][0][:], in_=ps[2][:], func=AF.Sigmoid), [psb[2]], [sg[1][1]])
                        S.mm(ps[3][:], [(wb_t[:, jj, :], OAall[:, jj, t0:t0 + 512]) for jj in range(4, 12)], [B_wb, B_OAall], [psb[3]])
                        S.op("dve", lambda e: e.tensor_tensor(out=m2[:], in0=sg[1][0][:], in1=ps[3][:], op=ALU.mult),
                             [sg[1][1], psb[3]], [B_m2])
                        S.op("dve", lambda e: e.tensor_tensor(out=MTc[:, t0:t0 + 512], in0=m1[:], in1=m2[:], op=ALU.add),
                             [B_m1, B_m2], [B_MTc])
                    S.dma("sp", mt_scr.rearrange("t p c k -> p c t k")[:, c], MTc[:].rearrange("p (t k) -> p t k", k=128),
                          [B_MTc], [B_mtscr])
                S.barrier()
        mid.close()
        if stage >= 4:
            with ExitStack() as pf:
                Wo, B_Wo = sb("Wo", [128, 16, D], BF16, pf)
                GMt, B_GMt = sb("GMt", [128, D], F32, pf)
                G2t, B_G2t = sb("G2t", [128, D], F32, pf)
                SH2t, B_SH2t = sb("SH2t", [128, D], F32, pf)
                mtt = [sb(f"mtt{i}", [128, 16, 128], BF16, pf) for i in range(2)]
                xr = [sb(f"xf{i}", [128, D], F32, pf) for i in range(2)]
                tmpf_r = [sb(f"tmpf{i}", [128, D], F32, pf) for i in range(2)]
                h2b_r = [sb(f"h2b{i}", [128, D], BF16, pf) for i in range(2)]
                h2t_r = [sb(f"h2tt{i}", [128, 16, 128], BF16, pf) for i in range(2)]
                junk_r = [sb(f"junkf{i}", [128, D], BF16, pf) for i in range(2)]
                ss_r = [sb(f"ssf{i}", [128, 4], F32, pf) for i in range(2)]
                m1_r = [sb(f"m1s{i}", [128, 8], F32, pf) for i in range(2)]
                yb_r = [sb(f"ybuf{i}", [128, D], F32, pf) for i in range(2)]
                yb2_b = [Buf("yb2a"), Buf("yb2b")]
                wrt, B_wrt = sb("wrt", [128, 16, NE], BF16, pf)
                rt = {k: sb("rt_" + k, shp, F32, pf) for k, shp in
                      (("sc", [128, 64]), ("ch", [128, 64]), ("m1", [128, 8]), ("eq", [128, 64]), ("ch2", [128, 64]),
                       ("m2", [128, 8]), ("gs", [128, 8]), ("t8", [128, 8]), ("gm", [128, 8]), ("chm", [128, 64]),
                       ("t8e", [128, 8]), ("sel", [128, 64]), ("gsel", [128, 64]), ("den", [128, 2]))}
                S.dma("pool", wrt[:], w_r, [], [B_wrt])
                for pc in range(8):
                    S.dma("pool", Wo[:, :, pc * 256:(pc + 1) * 256], w_out[pc], [], [B_Wo])
                S.dma("sp", GMt[:], modrep[2], [B_modrep[2]], [B_GMt])
                S.dma("sp", G2t[:], modrep[4], [B_modrep[4]], [B_G2t])
                S.dma("sp", SH2t[:], modrep[3], [B_modrep[3]], [B_SH2t])
                def f2_outproj(tile_i):
                    MTt, B_MTt = mtt[tile_i % 2]
                    S.dma("sp", MTt[:], mt_scr[tile_i], [B_mtscr], [B_MTt])
                    for n in range(4):
                        S.mm(ps[4 + n][:], [(MTt[:, kc, :], Wo[:, kc, n * 512:(n + 1) * 512]) for kc in range(16)],
                             [B_MTt, B_Wo], [psb[4 + n]])

                f2_outproj(0)
                for tile_i in range(16):
                    r0 = tile_i * 128
                    tmpf, B_tmpf = tmpf_r[tile_i % 2]
                    h2b, B_h2b = h2b_r[tile_i % 2]
                    h2t, B_h2tt = h2t_r[tile_i % 2]
                    junk, B_junk = junk_r[tile_i % 2]
                    ss, B_ss = ss_r[tile_i % 2]
                    m1, B_m1 = m1_r[tile_i % 2]
                    xin, B_x = xr[tile_i % 2]
                    S.dma("sp", xin[:], x_own[r0:r0 + 128, :], [], [B_x])
                    yb, B_yb = yb_r[tile_i % 2]
                    B_yb2 = yb2_b[tile_i % 2]
                    for n in range(4):
                        nsl = slice(n * 512, (n + 1) * 512)
                        if n % 2 == 0:
                            S.op("act", lambda e: e.copy(out=yb[:, nsl], in_=ps[4 + n][:]), [psb[4 + n]], [B_yb])
                        else:
                            S.op("dve", lambda e: e.tensor_copy(out=yb[:, nsl], in_=ps[4 + n][:]), [psb[4 + n]], [B_yb2])
                    S.op("dve", lambda e: e.memset(ss[:, 0:1], 0.0), [], [B_ss])
                    S.op("act", lambda e: e.activation(out=junk[:], in_=yb[:], func=AF.Square, accum_out=ss[:, 0:1]),
                         [B_yb, B_yb2], [B_junk, B_ss])
                    S.op("dve", lambda e: e.tensor_scalar(out=ss[:, 1:2], in0=ss[:, 0:1], scalar1=1.0 / D, scalar2=EPS,
                                                          op0=ALU.mult, op1=ALU.add), [B_ss], [B_ss])
                    S.op("act", lambda e: e.activation(out=ss[:, 2:3], in_=ss[:, 1:2], func=AF.Sqrt), [B_ss], [B_ss])
                    S.op("dve", lambda e: e.reciprocal(out=ss[:, 3:4], in_=ss[:, 2:3]), [B_ss], [B_ss])
                    S.op("dve", lambda e: e.scalar_tensor_tensor(out=tmpf[:], in0=yb[:], scalar=ss[:, 3:4], in1=GMt[:],
                                                                 op0=ALU.mult, op1=ALU.mult), [B_yb, B_yb2, B_ss, B_GMt], [B_tmpf])
                    S.op("dve", lambda e: e.tensor_tensor(out=xin[:], in0=xin[:], in1=tmpf[:], op=ALU.add), [B_x, B_tmpf], [B_x])
                    S.dma("pool", out_d[r0:r0 + 128, :], xin[:], [B_x], [B_out[tile_i]])
                    if debug:
                        S.dma("sp", dbg["d_x1"][r0:r0 + 128, :], xin[:], [B_x], [])
                    if tile_i < 15:
                        f2_outproj(tile_i + 1)
                    rstd = rms_rstd(xin[:], B_x, junk[:], B_junk, ss, B_ss, D)
                    S.op("dve", lambda e: e.scalar_tensor_tensor(out=tmpf[:], in0=xin[:], scalar=rstd, in1=G2t[:],
                                                                 op0=ALU.mult, op1=ALU.mult), [B_x, B_ss, B_G2t], [B_tmpf])
                    S.op("dve", lambda e: e.tensor_tensor(out=h2b[:], in0=tmpf[:], in1=SH2t[:], op=ALU.add), [B_tmpf, B_SH2t], [B_h2b])
                    for half in range(2):
                        pi = 2 + half
                        pv = ps[pi][:].bitcast(BF16).rearrange("p (k t) -> p k t", t=128)
                        S.deps("pe", [B_h2b, B_ident], [psb[pi]])
                        ins = None
                        for k in range(8):
                            kc = half * 8 + k
                            ins = nc.tensor.transpose(pv[:, k, :], h2b[:, kc * 128:(kc + 1) * 128], ident_bf[:])
                        S.pe_ticket_after(ins, [B_h2b, B_ident], [psb[pi]])
                        S.op("act", lambda e: e.copy(out=h2t[:, half * 8:half * 8 + 8, :], in_=pv), [psb[pi]], [B_h2tt])
                    S.dma("pool", h2t_scr[:, :, r0:r0 + 128], h2t[:], [B_h2tt], [B_h2t[tile_i]])
                    S.mm(ps[0][:, 0:NE], [(h2t[:, kc, :], wrt[:, kc, :]) for kc in range(16)], [B_h2tt, B_wrt], [psb[0]])
                    R = lambda k: rt[k][0]
                    RB = lambda k: rt[k][1]
                    v3 = lambda a: a.rearrange("p (g k) -> p g k", k=8)
                    S.op("act", lambda e: e.activation(out=R("sc")[:], in_=ps[0][:, 0:NE], func=AF.Sigmoid), [psb[0]], [RB("sc")])
                    S.op("dve", lambda e: e.tensor_tensor(out=R("ch")[:], in0=R("sc")[:], in1=rbias[:], op=ALU.add),
                         [RB("sc"), B_rbias], [RB("ch")])
                    S.op("dve", lambda e: e.tensor_reduce(out=R("m1")[:], in_=v3(R("ch")[:]), axis=AX.X, op=ALU.max),
                         [RB("ch")], [RB("m1")])
                    S.op("dve", lambda e: e.tensor_tensor(out=v3(R("eq")[:]), in0=v3(R("ch")[:]),
                                                          in1=R("m1")[:].unsqueeze(2).to_broadcast([128, 8, 8]), op=ALU.is_equal),
                         [RB("ch"), RB("m1")], [RB("eq")])
                    S.op("dve", lambda e: e.scalar_tensor_tensor(out=R("ch2")[:], in0=R("eq")[:], scalar=-1e9, in1=R("ch")[:],
                                                                 op0=ALU.mult, op1=ALU.add), [RB("eq"), RB("ch")], [RB("ch2")])
                    S.op("dve", lambda e: e.tensor_reduce(out=R("m2")[:], in_=v3(R("ch2")[:]), axis=AX.X, op=ALU.max),
                         [RB("ch2")], [RB("m2")])
                    S.op("dve", lambda e: e.tensor_tensor(out=R("gs")[:], in0=R("m1")[:], in1=R("m2")[:], op=ALU.add),
                         [RB("m1"), RB("m2")], [RB("gs")])
                    S.op("dve", lambda e: e.max(out=R("t8")[:], in_=R("gs")[:]), [RB("gs")], [RB("t8")])
                    S.op("dve", lambda e: e.tensor_scalar(out=R("gm")[:], in0=R("gs")[:], scalar1=R("t8")[:, 3:4], scalar2=None,
                                                          op0=ALU.is_ge), [RB("gs"), RB("t8")], [RB("gm")])
                    S.op("dve", lambda e: e.tensor_scalar(out=R("gm")[:], in0=R("gm")[:], scalar1=1e9, scalar2=-1e9,
                                                          op0=ALU.mult, op1=ALU.add), [RB("gm")], [RB("gm")])
                    S.op("dve", lambda e: e.tensor_tensor(out=v3(R("chm")[:]), in0=v3(R("ch")[:]),
                                                          in1=R("gm")[:].unsqueeze(2).to_broadcast([128, 8, 8]), op=ALU.add),
                         [RB("ch"), RB("gm")], [RB("chm")])
                    S.op("dve", lambda e: e.max(out=R("t8e")[:], in_=R("chm")[:]), [RB("chm")], [RB("t8e")])
                    S.op("dve", lambda e: e.tensor_scalar(out=R("sel")[:], in0=R("chm")[:], scalar1=R("t8e")[:, 7:8], scalar2=None,
                                                          op0=ALU.is_ge), [RB("chm"), RB("t8e")], [RB("sel")])
                    S.op("dve", lambda e: e.tensor_tensor(out=R("gsel")[:], in0=R("sel")[:], in1=R("sc")[:], op=ALU.mult),
                         [RB("sel"), RB("sc")], [RB("gsel")])
                    S.op("dve", lambda e: e.tensor_reduce(out=R("den")[:, 0:1], in_=R("gsel")[:], axis=AX.X, op=ALU.add),
                         [RB("gsel")], [RB("den")])
                    S.op("dve", lambda e: e.reciprocal(out=R("den")[:, 1:2], in_=R("den")[:, 0:1]), [RB("den")], [RB("den")])
                    S.op("dve", lambda e: e.tensor_scalar(out=Gt[:, tile_i, 0:NE], in0=R("gsel")[:], scalar1=R("den")[:, 1:2],
                                                          scalar2=2.5, op0=ALU.mult, op1=ALU.mult), [RB("gsel"), RB("den")], [B_Gt])
                if debug:
                    S.dma("sp", dbg["d_g"], Gt[:], [B_Gt], [])
                S.barrier()
        mid.close()
        if stage >= 5:
            with ExitStack() as pg:
                h2p, B_h2p = sb("h2p", [128, 16, 1024], BF16, pg)
                yacc, B_yacc = sb("yacc", [128, 8, D], F32, pg)
                ss, B_ss = sb("ssg", [128, 4], F32, pg)
                for p in range(2):
                    S.dma("sp", h2p[:], h2t_scr[:, :, p * 1024:(p + 1) * 1024], B_h2t, [B_h2p])
                    S.op("pool", lambda e: e.memset(yacc[:], 0.0), [], [B_yacc])
                    with ExitStack() as pw:
                        xgu = [(sb(f"xg{i}", [128, 16, 256], BF16, pw), sb(f"xu{i}", [128, 16, 256], BF16, pw)) for i in range(3)]
                        xdr = [sb(f"xd{i}", [128, 4, D], BF16, pw) for i in range(2)]
                        hid = [[sb(f"hid{a}{t}", [128, 4, 512], BF16, pw) for t in range(2)] for a in range(2)]
                        sgl = [sb(f"sgl{i}", [128, 512], F32, pw) for i in range(2)]
                        di = [0]

                        def emit_down(ex, regions):
                            xd, B_xd = xdr[ex % 2]
                            for (tg, i, n) in regions:
                                hd_, B_hd = hid[ex % 2][tg]
                                lt = tg * 4 + i
                                pi = 4 + (di[0] % 4)
                                di[0] += 1
                                nsl = slice(n * 512, (n + 1) * 512)
                                S.mm(ps[pi][:], [(hd_[:, kc, i * 128:(i + 1) * 128], xd[:, kc, nsl]) for kc in range(4)],
                                     [B_hd, B_xd], [psb[pi]])
                                S.op("dve", lambda e: e.scalar_tensor_tensor(out=yacc[:, lt, nsl], in0=ps[pi][:],
                                                                             scalar=Gt[:, p * 8 + lt, ex:ex + 1], in1=yacc[:, lt, nsl],
                                                                             op0=ALU.mult, op1=ALU.add),
                                     [psb[pi], B_Gt, B_yacc], [B_yacc])

                        allreg = [(tg, i, n) for tg in range(2) for i in range(4) for n in range(4)]
                        for ex in range(NE + 1):
                            gsrc = w_ge[ex] if ex < NE else w_gs
                            usrc = w_ue[ex] if ex < NE else w_us
                            dsrc = w_de[ex] if ex < NE else w_ds
                            slots = []
                            for hf in range(2):
                                (xg, B_xg), (xu, B_xu) = xgu[(2 * ex + hf) % 3]
                                hsl = slice(hf * 256, (hf + 1) * 256)
                                S.dma("pool", xg[:], gsrc[hf], [], [B_xg])
                                S.dma("pool", xu[:], usrc[hf], [], [B_xu])
                                slots.append(((xg, B_xg), (xu, B_xu)))
                            xd, B_xd = xdr[ex % 2]
                            S.dma("pool", xd[:], dsrc, [], [B_xd])
                            u = 0
                            for hf in range(2):
                                (xg, B_xg), (xu, B_xu) = slots[hf]
                                for tg in range(2):
                                    tsl = slice(tg * 512, (tg + 1) * 512)
                                    hd_, B_hd = hid[ex % 2][tg]
                                    for ch in range(2):
                                        csl = slice(ch * 128, (ch + 1) * 128)
                                        S.mm(ps[ch][:], [(xg[:, kc, csl], h2p[:, kc, tsl]) for kc in range(16)], [B_xg, B_h2p], [psb[ch]])
                                        S.mm(ps[2 + ch][:], [(xu[:, kc, csl], h2p[:, kc, tsl]) for kc in range(16)], [B_xu, B_h2p], [psb[2 + ch]])
                                    for ch in range(2):
                                        sg_, B_sg = sgl[ch]
                                        S.op("act", lambda e: e.activation(out=sg_[:], in_=ps[ch][:], func=AF.Silu), [psb[ch]], [B_sg])
                                        S.op("dve", lambda e: e.tensor_tensor(out=hd_[:, hf * 2 + ch, :], in0=sg_[:], in1=ps[2 + ch][:], op=ALU.mult),
                                             [B_sg, psb[2 + ch]], [B_hd])
                                    if ex > 0:
                                        emit_down(ex - 1, allreg[u * 8:(u + 1) * 8])
                                    u += 1
                        emit_down(NE, allreg)
                        S.barrier()
                    with ExitStack() as pz:
                        xin_r = [sb(f"xfin{i}", [128, D], F32, pz) for i in range(3)]
                        mrep, B_mrep = sb("mrepg", [128, D], F32, pz)
                        junk_r = [sb(f"junkg{i}", [128, D], BF16, pz) for i in range(2)]
                        ss_r = [sb(f"ssg{i}", [128, 4], F32, pz) for i in range(2)]
                        S.dma("sp", mrep[:], modrep[5], [B_modrep[5]], [B_mrep])
                        B_yt = [Buf(f"yacc_t{p}_{i}") for i in range(8)]
                        for lt in range(8):
                            r0 = (p * 8 + lt) * 128
                            B_yacc_l = B_yt[lt]
                            xin, B_x = xin_r[lt % 3]
                            junk, B_junk = junk_r[lt % 2]
                            ss, B_ss = ss_r[lt % 2]
                            S.dma("pool", xin[:], out_d[r0:r0 + 128, :], [B_out[p * 8 + lt]], [B_x])
                            rstd = rms_rstd(yacc[:, lt, :], B_yacc_l, junk[:], B_junk, ss, B_ss, D)
                            S.op("dve", lambda e: e.scalar_tensor_tensor(out=yacc[:, lt, :], in0=yacc[:, lt, :], scalar=rstd, in1=mrep[:],
                                                                         op0=ALU.mult, op1=ALU.add if False else ALU.mult),
                                 [B_yacc_l, B_ss, B_mrep], [B_yacc_l])
                            S.op("dve", lambda e: e.tensor_tensor(out=xin[:], in0=xin[:], in1=yacc[:, lt, :], op=ALU.add),
                                 [B_x, B_yacc_l], [B_x])
                            S.dma("sp", out_d[r0:r0 + 128, :], xin[:], [B_x], [B_out[p * 8 + lt]])
                        S.barrier()
        S.barrier()
    return nc


def _consts():
    ident = np.eye(128, dtype=np.float32)
    s = np.arange(128)[:, None]
    t = np.arange(128)[None, :]
    tri = ((s // 32 == t // 32) & (s <= t)).astype(np.float32)
    seg = np.ones((128, 512), np.float32)
    seg[:, ::32] = 0.0
    slopes = np.exp2(-8.0 * np.arange(1, 13, dtype=np.float64) / 12)
    alibi = np.zeros((12, 128, 256), np.float32)
    k = np.arange(128)[:, None].astype(np.float64)
    q = np.arange(128)[None, :].astype(np.float64)
    for h in range(12):
        d = DIL[h // 4]
        prev = np.where(k >= q, np.exp(-slopes[h] * d * (q + 128 - k)), 0.0)
        cur = np.where(k <= q, np.exp(-slopes[h] * d * (q - k)), 0.0)
        alibi[h, :, 0:128] = prev
        alibi[h, :, 128:256] = cur
    rowm = (np.arange(128)[:, None] // 32 == np.arange(4)[None, :]).astype(np.float32)
    return ident, tri, seg, alibi, rowm


def make_in_maps(inputs, cores, ned=NE):
    f = lambda a: np.ascontiguousarray(np.asarray(a, dtype=np.float32))
    x = f(inputs["x"])
    c = f(inputs["c"])
    ident, tri, seg, alibi, rowm = _consts()
    norms = np.stack([f(inputs["pre_norm_mix"])[0], f(inputs["post_norm_mix"])[0],
                      f(inputs["pre_norm_ffn"])[0], f(inputs["post_norm_ffn"])[0]], 0)
    lbl = f(inputs["hgrn_lb_logits"]).reshape(2, 8, 128).transpose(2, 0, 1).reshape(128, 16)
    hn = f(inputs["hgrn_norm"])[0].reshape(8, 128).T
    def kmaj(w, cw):
        K, C = w.shape
        return f(np.asarray(w).reshape(K // 128, 128, C // cw, cw).transpose(2, 1, 0, 3))

    def kmaj_e(w, cw):
        E, K, C = w.shape
        return f(np.asarray(w).reshape(E, K // 128, 128, C // cw, cw).transpose(0, 3, 2, 1, 4))

    wde = np.asarray(inputs["w_down_e"][0, :ned])
    shared = {
        "w_ada": kmaj(inputs["w_ada"][0], 512), "b_ada": f(inputs["b_ada"]), "norms": f(norms), "w_in": kmaj(inputs["w_in"][0], 128),
        "lbl": f(lbl), "hn": f(hn), "w_ba": kmaj(inputs["w_branch_attn"][0], 128), "w_bh": kmaj(inputs["w_branch_hgrn"][0], 128),
        "w_out": kmaj(inputs["w_out"][0], 256), "w_r": kmaj(inputs["w_router"][0], NE)[0], "rb": f(inputs["router_bias"]),
        "w_ge": kmaj_e(inputs["w_gate_e"][0, :ned], 256), "w_ue": kmaj_e(inputs["w_up_e"][0, :ned], 256),
        "w_de": f(wde.reshape(wde.shape[0], 4, 128, D).transpose(0, 2, 1, 3)),
        "w_gs": kmaj(inputs["w_gate_s"][0], 256), "w_us": kmaj(inputs["w_up_s"][0], 256),
        "w_ds": f(np.asarray(inputs["w_down_s"][0]).reshape(4, 128, D).transpose(1, 0, 2)),
        "ident": ident, "tri32": tri, "segmask": seg, "alibi": alibi, "rowm": rowm,
    }
    maps = []
    for core in cores:
        b, half = core // 2, core % 2
        m = dict(shared)
        m["x_own"] = f(x[b, half * TOK:(half + 1) * TOK])
        m["x_halo"] = f(x[b, 0:TOK]) if half == 1 else np.zeros((TOK, D), np.float32)
        m["flag"] = np.full((128, 1), float(half), np.float32)
        m["cb"] = f(c[b].reshape(16, 128).T)
        maps.append(m)
    return maps


def kernel(**inputs):
    nc = build()
    cores = list(range(8))
    maps = make_in_maps(inputs, cores)
    res = run_bass_kernel_spmd(nc, maps, core_ids=cores)
    out = np.zeros((4, 4096, D), np.float32)
    for core in cores:
        b, half = core // 2, core % 2
        out[b, half * TOK:(half + 1) * TOK] = res.results[core]["out"]
    return out
```

```python
import os
import numpy as np
from contextlib import ExitStack
import concourse.bass as bass
import concourse.mybir as mybir
from concourse.bass_utils import run_bass_kernel_spmd

F32 = mybir.dt.float32
BF16 = mybir.dt.bfloat16
AF = mybir.ActivationFunctionType
ALU = mybir.AluOpType
AX = mybir.AxisListType

D = 2048
TOK = 2048
EPS = 1e-6
DIL = (1, 4, 16)
NE = 64
COL_QA, COL_KA, COL_VA = 0, 1536, 3072
COL_QR, COL_FR, COL_IR, COL_GR = 4608, 5632, 6656, 7680
COL_GA, COL_GB = 8704, 10752


class Buf:
    __slots__ = ("name", "w", "r")

    def __init__(self, name):
        self.name = name
        self.w = None
        self.r = {}


class Sched:
    ND = 8

    def __init__(self, nc, es):
        self.nc = nc
        self.engs = {"pe": nc.tensor, "act": nc.scalar, "dve": nc.vector, "pool": nc.gpsimd, "sp": nc.sync}
        self.sems = {}
        self.ccnt = {}
        for e in ("pe", "act", "dve", "pool"):
            self.sems["c_" + e] = es.enter_context(nc.semaphore("c_" + e))
            self.ccnt[e] = 0
        self.dcnt = {}
        self.drr = {}
        for q in ("sp", "pool"):
            for i in range(self.ND):
                self.sems[f"d_{q}{i}"] = es.enter_context(nc.semaphore(f"d_{q}{i}"))
                self.dcnt[(q, i)] = 0
            self.drr[q] = 0
        self.waited = {}

    def _wait(self, eng, t):
        if t is None:
            return
        sid, val = t
        if eng == "pe" and sid == "c_pe":
            return
        key = (eng, sid)
        if self.waited.get(key, 0) >= val:
            return
        self.engs[eng].wait_ge(self.sems[sid], val)
        self.waited[key] = val

    def deps(self, eng, reads, writes):
        for b in reads:
            self._wait(eng, b.w)
        for b in writes:
            self._wait(eng, b.w)
            for sid, val in b.r.items():
                self._wait(eng, (sid, val))

    def commit(self, t, reads, writes):
        for b in reads:
            if b.r.get(t[0], 0) < t[1]:
                b.r[t[0]] = t[1]
        for b in writes:
            b.w = t
            b.r = {}

    def op(self, eng, fn, reads=(), writes=()):
        self.deps(eng, reads, writes)
        ins = fn(self.engs[eng])
        self.ccnt[eng] += 1
        ins.then_inc(self.sems["c_" + eng], 1)
        self.commit(("c_" + eng, self.ccnt[eng]), reads, writes)

    def mm(self, out, pairs, reads, writes, start=True, stop=True, ticket=True, **kw):
        self.deps("pe", reads, writes)
        n = len(pairs)
        ins = None
        for i, (l, r) in enumerate(pairs):
            ins = self.nc.tensor.matmul(out, l, r, start=(start and i == 0), stop=(stop and i == n - 1), **kw)
        if ticket:
            self.ccnt["pe"] += 1
            ins.then_inc(self.sems["c_pe"], 1)
            self.commit(("c_pe", self.ccnt["pe"]), reads, writes)

    def pe_ticket_after(self, ins, reads, writes):
        self.ccnt["pe"] += 1
        ins.then_inc(self.sems["c_pe"], 1)
        self.commit(("c_pe", self.ccnt["pe"]), reads, writes)

    def dma(self, q, out, in_, reads, writes):
        i = self.drr[q]
        self.drr[q] = (i + 1) % self.ND
        sid = f"d_{q}{i}"
        prev = self.dcnt[(q, i)]
        if prev > 0:
            self._wait(q, (sid, 16 * prev))
        self.deps(q, reads, writes)
        self.engs[q].dma_start(out=out, in_=in_).then_inc(self.sems[sid], 16)
        self.dcnt[(q, i)] += 1
        self.commit((sid, 16 * self.dcnt[(q, i)]), reads, writes)

    def barrier(self):
        tickets = [("c_" + e, self.ccnt[e]) for e in ("pe", "act", "dve", "pool") if self.ccnt[e] > 0]
        for (q, i), c in self.dcnt.items():
            if c > 0:
                tickets.append((f"d_{q}{i}", 16 * c))
        for e in ("pe", "act", "dve", "pool", "sp"):
            for t in tickets:
                self._wait(e, t)


def build(stage=99, debug=False):
    nc = bass.Bass("TRN2", target_bir_lowering=False)

    def din(name, shape, dt=F32):
        return nc.dram_tensor(name, list(shape), dt, kind="ExternalInput").ap()

    x_own = din("x_own", [TOK, D])
    x_halo = din("x_halo", [TOK, D])
    flag_d = din("flag", [128, 1])
    cb_d = din("cb", [128, 16])
    w_ada = din("w_ada", [24, 128, 16, 512])
    b_ada = din("b_ada", [1, 6 * D])
    norms_d = din("norms", [4, D])
    w_in = din("w_in", [100, 128, 16, 128])
    lbl_d = din("lbl", [128, 16])
    hn_d = din("hn", [128, 8])
    w_ba = din("w_ba", [16, 128, 4, 128])
    w_bh = din("w_bh", [16, 128, 8, 128])
    w_out = din("w_out", [8, 128, 16, 256])
    w_r = din("w_r", [128, 16, NE])
    rb_d = din("rb", [1, NE])
    NED = NE if stage >= 5 else 1
    w_ge = din("w_ge", [NED, 2, 128, 16, 256])
    w_ue = din("w_ue", [NED, 2, 128, 16, 256])
    w_de = din("w_de", [NED, 128, 4, D])
    w_gs = din("w_gs", [2, 128, 16, 256])
    w_us = din("w_us", [2, 128, 16, 256])
    w_ds = din("w_ds", [128, 4, D])
    ident_d = din("ident", [128, 128])
    tri_d = din("tri32", [128, 128])
    seg_d = din("segmask", [128, 512])
    alibi_d = din("alibi", [12, 128, 256])
    rowm_d = din("rowm", [128, 4])
    out_d = nc.dram_tensor("out", [TOK, D], F32, kind="ExternalOutput").ap()
    modrep = nc.dram_tensor("modrep", [6, 128, D], F32).ap()
    oa_scr = nc.dram_tensor("oa_scr", [128, 12, TOK], BF16).ap()
    h2t_scr = nc.dram_tensor("h2t_scr", [128, 16, TOK], BF16).ap()
    mt_scr = nc.dram_tensor("mt_scr", [16, 128, 16, 128], BF16).ap()
    dbg = {}
    if debug:
        for nm, shp in (("d_h", [128, 16, TOK]), ("d_oa", [128, 12, TOK]), ("d_x1", [TOK, D]), ("d_g", [128, 16, 65])):
            dbg[nm] = nc.dram_tensor(nm, shp, F32, kind="ExternalOutput").ap()

    B_out = [Buf(f"out{i}") for i in range(16)]
    B_modrep = [Buf(f"modrep{i}") for i in range(6)]
    B_oascr = [Buf(f"oascr{i}") for i in range(12)]
    B_h2t = [Buf(f"h2t{i}") for i in range(16)]
    B_mtscr = Buf("mtscr")

    with ExitStack() as es:
        S = Sched(nc, es)

        uid = [0]

        def sb(name, shape, dt, stack=es):
            uid[0] += 1
            name = f"{name}_{uid[0]}"
            t = stack.enter_context(nc.sbuf_tensor(name, list(shape), dt))
            return t, Buf(name)

        ps = []
        psb = []
        for i in range(8):
            ps.append(es.enter_context(nc.psum_tensor(f"ps{i}", [128, 512], F32)))
            psb.append(Buf(f"ps{i}"))

        ident_bf, B_ident = sb("ident_bf", [128, 128], BF16)
        tri, B_tri = sb("tri", [128, 128], F32)
        segm, B_segm = sb("segm", [128, 512], F32)
        ones_bf, B_ones = sb("ones_bf", [128, 128], BF16)
        flag, B_flag = sb("flag_t", [128, 1], F32)
        lbl, B_lbl = sb("lbl_t", [128, 16], F32)
        lb, B_lb = sb("lb", [128, 8], F32)
        oml, B_oml = sb("oml", [128, 8], F32)
        hn, B_hn = sb("hn_t", [128, 8], F32)
        rbias, B_rbias = sb("rbias", [128, NE], F32)
        Gt, B_Gt = sb("Gt", [128, 16, 65], F32)
        S.dma("pool", ident_bf[:], ident_d, [], [B_ident])
        S.dma("sp", tri[:], tri_d, [], [B_tri])
        rowm, B_rowm = sb("rowm", [128, 4], F32)
        S.dma("sp", rowm[:], rowm_d, [], [B_rowm])
        S.dma("sp", segm[:], seg_d, [], [B_segm])
        S.dma("sp", flag[:], flag_d, [], [B_flag])
        S.dma("sp", lbl[:], lbl_d, [], [B_lbl])
        S.dma("sp", hn[:], hn_d, [], [B_hn])
        S.dma("sp", rbias[:], rb_d[0:1, :].to_broadcast([128, NE]), [], [B_rbias])
        S.op("dve", lambda e: e.memset(ones_bf[:], 1.0), [], [B_ones])
        S.op("dve", lambda e: e.memset(Gt[:], 1.0), [], [B_Gt])
        S.op("dve", lambda e: e.tensor_tensor(out=lb[:], in0=lbl[:, 0:8], in1=lbl[:, 8:16], op=ALU.subtract), [B_lbl], [B_lb])
        S.op("act", lambda e: e.activation(out=lb[:], in_=lb[:], func=AF.Sigmoid), [B_lb], [B_lb])
        S.op("dve", lambda e: e.tensor_scalar(out=oml[:], in0=lb[:], scalar1=-1.0, scalar2=1.0, op0=ALU.mult, op1=ALU.add),
             [B_lb], [B_oml])

        NWR = 6
        mid = ExitStack()
        wring = [sb(f"wring{i}", [128, 16, 128], BF16, mid) for i in range(NWR)]
        big, B_hT = sb("hTbig", [128, 16, TOK], BF16, mid)
        hp = ExitStack()
        wr_i = [0]

        def load_wchunk(col0):
            t, b = wring[wr_i[0] % NWR]
            wr_i[0] += 1
            S.dma("pool", t[:], w_in[col0 // 128], [], [b])
            return t, b

        def inproj_fm(w, hT, B_hT, t0, n, pst, B_ps, off=0):
            wt, wb = w
            S.mm(pst[:, off:off + n], [(wt[:, kc, :], hT[:, kc, t0:t0 + n]) for kc in range(16)], [wb, B_hT], [B_ps])

        def inproj_tm(w, hT, B_hT, tsl, pst_ap, B_ps):
            wt, wb = w
            S.mm(pst_ap, [(hT[:, kc, tsl], wt[:, kc, :]) for kc in range(16)], [wb, B_hT], [B_ps])

        def rms_rstd(src_ap, B_src, junk, B_junk, ss, B_ss, nfeat):
            S.op("dve", lambda e: e.memset(ss[:, 0:1], 0.0), [], [B_ss])
            S.op("act", lambda e: e.activation(out=junk, in_=src_ap, func=AF.Square, accum_out=ss[:, 0:1]),
                 [B_src], [B_junk, B_ss])
            S.op("dve", lambda e: e.tensor_scalar(out=ss[:, 1:2], in0=ss[:, 0:1], scalar1=1.0 / nfeat, scalar2=EPS,
                                                  op0=ALU.mult, op1=ALU.add), [B_ss], [B_ss])
            S.op("act", lambda e: e.activation(out=ss[:, 2:3], in_=ss[:, 1:2], func=AF.Sqrt), [B_ss], [B_ss])
            S.op("dve", lambda e: e.reciprocal(out=ss[:, 3:4], in_=ss[:, 2:3]), [B_ss], [B_ss])
            return ss[:, 3:4]

        with ExitStack() as pa:
          if stage >= -1:
                sc, B_sc = sb("sc", [128, 16], F32, pa)
                screp, B_screp = sb("screp", [128, 16, 128], BF16, pa)
                modt, B_modt = sb("modt", [128, D], F32, pa)
                nrm, B_nrm = sb("nrm", [128, D], F32, pa)
                bad, B_bad = sb("bad", [128, D], F32, pa)
                wad = [sb(f"wad{i}", [128, 16, 512], BF16, pa) for i in range(2)]
                S.dma("sp", sc[:], cb_d, [], [B_sc])
                S.op("act", lambda e: e.activation(out=sc[:], in_=sc[:], func=AF.Silu), [B_sc], [B_sc])
                S.op("dve", lambda e: e.tensor_copy(out=screp[:], in_=sc[:].unsqueeze(2).to_broadcast([128, 16, 128])),
                     [B_sc], [B_screp])
                nrm_of = {1: 0, 2: 1, 4: 2, 5: 3}
                for k in range(6):
                    S.dma("sp", bad[:], b_ada[0:1, k * D:(k + 1) * D].to_broadcast([128, D]), [], [B_bad])
                    if k in nrm_of:
                        S.dma("sp", nrm[:], norms_d[nrm_of[k]:nrm_of[k] + 1, :].to_broadcast([128, D]), [], [B_nrm])
                    for n in range(4):
                        wt, wb = wad[(k * 4 + n) % 2]
                        c0 = k * D + n * 512
                        S.dma("pool", wt[:], w_ada[k * 4 + n], [], [wb])
                        pi = (k * 4 + n) % 2
                        S.mm(ps[pi][:], [(screp[:, kc, :], wt[:, kc, :]) for kc in range(16)], [wb, B_screp], [psb[pi]])
                        S.op("dve", lambda e: e.tensor_tensor(out=modt[:, n * 512:(n + 1) * 512], in0=ps[pi][:],
                                                              in1=bad[:, n * 512:(n + 1) * 512], op=ALU.add),
                             [psb[pi], B_bad], [B_modt])
                    if k in (1, 4):
                        S.op("dve", lambda e: e.scalar_tensor_tensor(out=modt[:], in0=modt[:], scalar=1.0, in1=nrm[:],
                                                                     op0=ALU.add, op1=ALU.mult), [B_modt, B_nrm], [B_modt])
                    if k in (2, 5):
                        S.op("dve", lambda e: e.tensor_tensor(out=modt[:], in0=modt[:], in1=nrm[:], op=ALU.mult),
                             [B_modt, B_nrm], [B_modt])
                    S.dma("sp", modrep[k], modt[:], [B_modt], [B_modrep[k]])
                S.barrier()

        def make_hT(xsrc, hT, B_hT, pstk):
            G1, B_G1 = sb("G1", [128, D], F32, pstk)
            SH1, B_SH1 = sb("SH1", [128, D], F32, pstk)
            xr = [sb(f"xin{i}", [128, D], F32, pstk) for i in range(2)]
            hb, B_hb = sb("hb", [128, D], BF16, pstk)
            junk, B_junk = sb("junk", [128, D], BF16, pstk)
            ss, B_ss = sb("ss", [128, 4], F32, pstk)
            S.dma("sp", G1[:], modrep[1], [B_modrep[1]], [B_G1])
            S.dma("sp", SH1[:], modrep[0], [B_modrep[0]], [B_SH1])
            for t in range(16):
                xin, B_x = xr[t % 2]
                S.dma("sp", xin[:], xsrc[t * 128:(t + 1) * 128, :], [], [B_x])
                rstd = rms_rstd(xin[:], B_x, junk[:], B_junk, ss, B_ss, D)
                S.op("dve", lambda e: e.scalar_tensor_tensor(out=xin[:], in0=xin[:], scalar=rstd, in1=G1[:],
                                                             op0=ALU.mult, op1=ALU.mult), [B_x, B_ss, B_G1], [B_x])
                S.op("dve", lambda e: e.tensor_tensor(out=hb[:], in0=xin[:], in1=SH1[:], op=ALU.add), [B_x, B_SH1], [B_hb])
                for half in range(2):
                    pi = 2 + half
                    pv = ps[pi][:].bitcast(BF16).rearrange("p (k t) -> p k t", t=128)
                    S.deps("pe", [B_hb, B_ident], [psb[pi]])
                    ins = None
                    for k in range(8):
                        kc = half * 8 + k
                        ins = nc.tensor.transpose(pv[:, k, :], hb[:, kc * 128:(kc + 1) * 128], ident_bf[:])
                    S.pe_ticket_after(ins, [B_hb, B_ident], [psb[pi]])
                    eng = "act" if half == 0 else "dve"
                    if eng == "act":
                        S.op("act", lambda e: e.copy(out=hT[:, half * 8:half * 8 + 8, t * 128:(t + 1) * 128], in_=pv),
                             [psb[pi]], [B_hT])
                    else:
                        S.op("dve", lambda e: e.tensor_copy(out=hT[:, half * 8:half * 8 + 8, t * 128:(t + 1) * 128], in_=pv),
                             [psb[pi]], [B_hT])

        KH3, B_KH3 = sb("KH3", [128, 4, 2048], BF16, hp)
        VH3, B_VH3 = sb("VH3", [128, 4, 16, 128], BF16, hp)
        KH2, B_KH2 = sb("KH2", [128, 4, 512], BF16, hp)
        VH2, B_VH2 = sb("VH2", [128, 4, 4, 128], BF16, hp)
        KH1, B_KH1 = sb("KH1", [128, 4, 128], BF16, hp)
        VH1, B_VH1 = sb("VH1", [128, 4, 1, 128], BF16, hp)
        SH, B_SH = sb("SHst", [128, 8, 128], F32, hp)
        KH = (KH1, KH2, KH3)
        B_KH = (B_KH1, B_KH2, B_KH3)
        VH = (VH1, VH2, VH3)
        B_VH = (B_VH1, B_VH2, B_VH3)


        def hgrn_front(hh, hT, t0, own, wts, tmp):
            wq, wf, wi, wg = wts
            (f1, B_f1), (f2, B_f2), (f3, B_f3), (f4, B_f4), (f5, B_f5) = tmp["f"]
            (kdT, B_kdT), (kbT, B_kbT), (qbT, B_qbT) = tmp["kq"]
            (kdk, B_kdk), (Vtok, B_Vtok), (eL, B_eL), (AT, B_AT) = tmp["m"]
            inproj_fm(wf, hT, B_hT, t0, 512, ps[0], psb[0])
            if own:
                inproj_fm(wq, hT, B_hT, t0, 512, ps[1], psb[1])
            pv3 = ps[3][:].rearrange("p (k t) -> p k t", t=128)
            for i in range(4):
                S.mm(pv3[:, i, :], [(hT[:, kc, t0 + i * 128:t0 + (i + 1) * 128], wi[0][:, kc, :]) for kc in range(16)],
                     [wi[1], B_hT], [psb[3]])
            if own:
                inproj_fm(wg, hT, B_hT, t0, 512, ps[7], psb[7])
            S.op("act", lambda e: e.activation(out=f1[:], in_=ps[0][:], func=AF.Sigmoid), [psb[0]], [B_f1])
            S.op("dve", lambda e: e.tensor_scalar(out=f2[:], in0=f1[:], scalar1=oml[:, hh:hh + 1], scalar2=lb[:, hh:hh + 1],
                                                  op0=ALU.mult, op1=ALU.add), [B_f1, B_oml, B_lb], [B_f2])
            yield
            S.op("act", lambda e: e.activation(out=f1[:], in_=f2[:], func=AF.Ln), [B_f2], [B_f1])
            S.op("dve", lambda e: e.tensor_tensor_scan(out=f3[:], data0=segm[:], data1=f1[:], initial=0.0,
                                                       op0=ALU.mult, op1=ALU.add), [B_f1, B_segm], [B_f3])
            b3 = f3[:].rearrange("p (c k) -> p c k", k=32)
            S.op("dve", lambda e: e.tensor_tensor(out=f1[:].rearrange("p (c k) -> p c k", k=32),
                                                  in0=b3[:, :, 31:32].to_broadcast([128, 16, 32]), in1=b3, op=ALU.subtract),
                 [B_f3], [B_f1])
            yield
            S.op("act", lambda e: e.activation(out=f4[:], in_=f1[:], func=AF.Exp), [B_f1], [B_f4])
            if own:
                S.op("act", lambda e: e.activation(out=f5[:], in_=f3[:], func=AF.Exp, scale=-1.0), [B_f3], [B_f5])
            S.op("act", lambda e: e.activation(out=eL[:].unsqueeze(2), in_=b3[:, :, 31:32], func=AF.Exp), [B_f3], [B_eL])
            S.op("dve", lambda e: e.tensor_scalar(out=f2[:], in0=f2[:], scalar1=-1.0, scalar2=1.0, op0=ALU.mult, op1=ALU.add),
                 [B_f2], [B_f2])
            S.op("dve", lambda e: e.tensor_tensor(out=kdT[:], in0=f2[:], in1=f4[:], op=ALU.mult), [B_f2, B_f4], [B_kdT])
            S.op("act", lambda e: e.copy(out=Vtok[:], in_=pv3), [psb[3]], [B_Vtok])
            Vm, B_Vm = tmp["vm"]
            for c in range(4):
                S.op("act", lambda e: e.activation(out=Vm[:, c, :, :], in_=Vtok[:], func=AF.Copy, scale=rowm[:, c:c + 1]),
                     [B_Vtok, B_rowm], [B_Vm])
            yield
            if own:
                S.op("dve", lambda e: e.tensor_tensor(out=kbT[:], in0=f2[:], in1=f5[:], op=ALU.mult), [B_f2, B_f5], [B_kbT])
                S.op("act", lambda e: e.activation(out=f4[:], in_=f3[:], func=AF.Exp), [B_f3, B_kdT], [B_f4])
                S.op("dve", lambda e: e.tensor_tensor(out=qbT[:], in0=ps[1][:], in1=f4[:], op=ALU.mult), [psb[1], B_f4], [B_qbT])
                S.op("act", lambda e: e.activation(out=f5[:], in_=ps[7][:], func=AF.Silu), [psb[7], B_kbT], [B_f5])

        def hgrn_back(hh, t0, own, tmp, Sst, B_S, Sbf, curbox, ORT=None, B_ORT=None):
            cur = curbox[0]
            (f1, B_f1), (f2, B_f2), (f3, B_f3), (f4, B_f4), (f5, B_f5) = tmp["f"]
            (kdT, B_kdT), (kbT, B_kbT), (qbT, B_qbT) = tmp["kq"]
            (kdk, B_kdk), (Vtok, B_Vtok), (eL, B_eL), (AT, B_AT) = tmp["m"]
            Vm, B_Vm = tmp["vm"]
            pv = ps[2][:].bitcast(BF16)[:, 0:512].rearrange("p (k t) -> p k t", t=128)
            pA = ps[2][:, 256:384]
            S.deps("pe", [B_kdT, B_ident], [psb[2]])
            ins = None
            for i in range(4):
                ins = nc.tensor.transpose(pv[:, i, :], kdT[:, i * 128:(i + 1) * 128], ident_bf[:])
            S.pe_ticket_after(ins, [B_kdT, B_ident], [psb[2]])
            S.op("act", lambda e: e.copy(out=kdk[:], in_=pv), [psb[2]], [B_kdk])
            for i in range(4):
                if i > 0:
                    yield
                tsl = slice(i * 128, (i + 1) * 128)
                pU = ps[5 + (i % 2)]
                B_pU = psb[5 + (i % 2)]
                pUv = pU[:].rearrange("p (k t) -> p k t", t=128)
                for c in range(4):
                    S.mm(pUv[:, c, :], [(kdk[:, i, :], Vm[:, c, i, :])], [B_kdk, B_Vm], [B_pU])
                if own:
                    S.mm(pA, [(kbT[:, tsl], qbT[:, tsl])], [B_kbT, B_qbT], [psb[2]])
                    S.op("dve", lambda e: e.tensor_tensor(out=AT[:], in0=pA, in1=tri[:], op=ALU.mult),
                         [psb[2], B_tri], [B_AT])
                    S.mm(ps[4][:, tsl], [(Vtok[:, i, :], AT[:])], [B_Vtok, B_AT], [psb[4]], stop=False, skip_group_check=True)
                for c in range(4):
                    if own:
                        csl = slice(i * 128 + 32 * c, i * 128 + 32 * c + 32)
                        S.mm(ps[4][:, csl], [(Sbf[cur][0][:], qbT[:, csl])], [Sbf[cur][1], B_qbT], [psb[4]],
                             start=False, stop=True, skip_group_check=True)
                    S.op("dve", lambda e: e.scalar_tensor_tensor(out=Sst, in0=Sst, scalar=eL[:, i * 4 + c:i * 4 + c + 1],
                                                                 in1=pUv[:, c, :], op0=ALU.mult, op1=ALU.add),
                         [B_S, B_eL, B_pU], [B_S])
                    cur = (cur + 1) % len(Sbf)
                    if own:
                        S.op("dve", lambda e: e.tensor_copy(out=Sbf[cur][0][:], in_=Sst), [B_S], [Sbf[cur][1]])
            if own:
                (sq, B_sq), (o1, B_o1) = tmp["o"]
                S.op("act", lambda e: e.activation(out=sq[:], in_=ps[4][:], func=AF.Square), [psb[4]], [B_sq])
                S.mm(ps[0][:], [(ones_bf[:], sq[:])], [B_ones, B_sq], [psb[0]])
                S.op("dve", lambda e: e.tensor_scalar(out=f1[:], in0=ps[0][:], scalar1=1.0 / 128, scalar2=EPS,
                                                      op0=ALU.mult, op1=ALU.add), [psb[0]], [B_f1])
                S.op("act", lambda e: e.activation(out=f1[:], in_=f1[:], func=AF.Sqrt), [B_f1], [B_f1])
                S.op("dve", lambda e: e.reciprocal(out=f1[:], in_=f1[:]), [B_f1], [B_f1])
                S.op("dve", lambda e: e.tensor_tensor(out=o1[:], in0=ps[4][:], in1=f1[:], op=ALU.mult), [psb[4], B_f1], [B_o1])
                S.op("dve", lambda e: e.scalar_tensor_tensor(out=ORT[:, t0:t0 + 512], in0=o1[:], scalar=hn[:, hh:hh + 1],
                                                             in1=f5[:], op0=ALU.mult, op1=ALU.mult),
                     [B_o1, B_hn, B_f5], [B_ORT])
            curbox[0] = cur
            yield

        def hgrn_tmp(stk):
            return {
                "f": [sb(f"hf{i}", [128, 512], F32, stk) for i in range(5)],
                "kq": [sb(f"hk{i}", [128, 512], BF16, stk) for i in range(3)],
                "m": [sb("kdk", [128, 4, 128], BF16, stk), sb("Vtok", [128, 4, 128], BF16, stk),
                      sb("eL", [128, 16], F32, stk), sb("AT", [128, 128], BF16, stk)],
                "o": [sb("sq", [128, 512], BF16, stk), sb("o1", [128, 512], F32, stk)],
                "vm": sb("Vm", [128, 4, 4, 128], BF16, stk),
            }

        with ExitStack() as ph:
          if stage >= 0:
                with ExitStack() as ph0:
                    make_hT(x_halo, big, B_hT, ph0)
                    S.barrier()
                cpi = 0
                HSTOP = int(os.environ.get("HSTOP", "9"))
                for g in (range(3) if HSTOP >= 2 else []):
                    d = DIL[g]
                    span = 128 * d
                    for j in range(4):
                        hh = g * 4 + j
                        wk = load_wchunk(COL_KA + hh * 128)
                        wv = load_wchunk(COL_VA + hh * 128)
                        step = min(512, span)
                        for u0 in range(TOK - span, TOK, step):
                            pi = cpi % 2
                            cpi += 1
                            inproj_fm(wk, big, B_hT, u0, step, ps[pi], psb[pi])
                            o0 = u0 - (TOK - span)
                            S.op("act", lambda e: e.copy(out=KH[g][:, j, o0:o0 + step], in_=ps[pi][:, 0:step]), [psb[pi]], [B_KH[g]])
                        for r0 in range(0, d, 4):
                            nb = min(4, d - r0)
                            pi = 2 + (cpi % 2)
                            cpi += 1
                            pv = ps[pi][:].rearrange("p (k t) -> p k t", t=128)
                            for rr in range(nb):
                                r = r0 + rr
                                tsl = slice(TOK - span + r, TOK, d)
                                S.mm(pv[:, rr, :], [(big[:, kc, tsl], wv[0][:, kc, :]) for kc in range(16)], [wv[1], B_hT], [psb[pi]],
                                     ticket=True)
                            S.op("dve", lambda e: e.tensor_copy(out=VH[g][:, j, r0:r0 + nb, :], in_=pv[:, 0:nb, :]), [psb[pi]], [B_VH[g]])
                with ExitStack() as phh:
                    gfull, B_gf = sb("gfull", [128, TOK], F32, phh)
                    omf, B_omf = sb("omfull", [128, TOK], F32, phh)
                    Bc, B_Bc = sb("Bcum", [128, TOK], F32, phh)
                    kdTf, B_kdTf = sb("kdTf", [128, TOK], BF16, phh)
                    kdtok, B_kdtok = sb("kdtok", [128, 16, 128], BF16, phh)
                    Vth, B_Vth = sb("Vth", [128, 16, 128], BF16, phh)
                    fs = [sb(f"fsig{i}", [128, 512], F32, phh) for i in range(2)]
                    ones5, B_ones5 = sb("ones5", [128, 512], F32, phh)
                    S.op("dve", lambda e: e.memset(ones5[:], 1.0), [], [B_ones5])
                    S.op("dve", lambda e: e.memset(SH[:], 0.0), [], [B_SH])
                    for hh in (range(8) if HSTOP >= 3 else []):
                        wf = load_wchunk(COL_FR + hh * 128)
                        wi = load_wchunk(COL_IR + hh * 128)
                        for seg in range(4):
                            t0 = seg * 512
                            tsl = slice(t0, t0 + 512)
                            pf_, B_pf = ps[seg % 2], psb[seg % 2]
                            f1, B_f1 = fs[seg % 2]
                            inproj_fm(wf, big, B_hT, t0, 512, pf_, B_pf)
                            S.op("act", lambda e: e.activation(out=f1[:], in_=pf_[:], func=AF.Sigmoid), [B_pf], [B_f1])
                            S.op("dve", lambda e: e.tensor_scalar(out=omf[:, tsl], in0=f1[:], scalar1=oml[:, hh:hh + 1],
                                                                  scalar2=lb[:, hh:hh + 1], op0=ALU.mult, op1=ALU.add),
                                 [B_f1, B_oml, B_lb], [B_omf])
                            S.op("act", lambda e: e.activation(out=gfull[:, tsl], in_=omf[:, tsl], func=AF.Ln), [B_omf], [B_gf])
                            S.op("dve", lambda e: e.tensor_scalar(out=omf[:, tsl], in0=omf[:, tsl], scalar1=-1.0, scalar2=1.0,
                                                                  op0=ALU.mult, op1=ALU.add), [B_omf, B_gf], [B_omf])
                            init = 0.0 if seg == 0 else Bc[:, t0 - 1:t0]
                            S.op("dve", lambda e: e.tensor_tensor_scan(out=Bc[:, tsl], data0=ones5[:], data1=gfull[:, tsl], initial=init,
                                                                       op0=ALU.mult, op1=ALU.add), [B_gf, B_ones5, B_Bc], [B_Bc])
                            pvb = ps[2 + seg % 2]
                            B_pvb = psb[2 + seg % 2]
                            pv3 = pvb[:].rearrange("p (k t) -> p k t", t=128)
                            for i in range(4):
                                S.mm(pv3[:, i, :], [(big[:, kc, t0 + i * 128:t0 + (i + 1) * 128], wi[0][:, kc, :]) for kc in range(16)],
                                     [wi[1], B_hT], [B_pvb])
                            if seg % 2 == 0:
                                S.op("act", lambda e: e.copy(out=Vth[:, seg * 4:seg * 4 + 4, :], in_=pv3), [B_pvb], [B_Vth])
                            else:
                                S.op("dve", lambda e: e.tensor_copy(out=Vth[:, seg * 4:seg * 4 + 4, :], in_=pv3), [B_pvb], [B_Vth])
                        S.op("dve", lambda e: e.tensor_tensor(out=gfull[:], in0=Bc[:, TOK - 1:TOK].to_broadcast([128, TOK]), in1=Bc[:],
                                                              op=ALU.subtract), [B_Bc, B_gf], [B_gf])
                        S.op("act", lambda e: e.activation(out=gfull[:], in_=gfull[:], func=AF.Exp), [B_gf], [B_gf])
                        S.op("dve", lambda e: e.tensor_tensor(out=kdTf[:], in0=omf[:], in1=gfull[:], op=ALU.mult), [B_omf, B_gf], [B_kdTf])
                        for half in range(2):
                            pi = 4 + half
                            pv = ps[pi][:].bitcast(BF16).rearrange("p (k t) -> p k t", t=128)
                            S.deps("pe", [B_kdTf, B_ident], [psb[pi]])
                            ins = None
                            for k in range(8):
                                i = half * 8 + k
                                ins = nc.tensor.transpose(pv[:, k, :], kdTf[:, i * 128:(i + 1) * 128], ident_bf[:])
                            S.pe_ticket_after(ins, [B_kdTf, B_ident], [psb[pi]])
                            if half == 0:
                                S.op("act", lambda e: e.copy(out=kdtok[:, 0:8, :], in_=pv), [psb[pi]], [B_kdtok])
                            else:
                                S.op("dve", lambda e: e.tensor_copy(out=kdtok[:, 8:16, :], in_=pv), [psb[pi]], [B_kdtok])
                        S.mm(ps[6][:, 0:128], [(kdtok[:, i, :], Vth[:, i, :]) for i in range(16)], [B_kdtok, B_Vth], [psb[6]])
                        S.op("dve", lambda e: e.tensor_copy(out=SH[:, hh, :], in_=ps[6][:, 0:128]), [psb[6]], [B_SH])
                    S.op("dve", lambda e: e.tensor_scalar(out=SH[:], in0=SH[:], scalar1=flag[:, 0:1], scalar2=None, op0=ALU.mult),
                         [B_SH, B_flag], [B_SH])
                S.barrier()

        with ExitStack() as pb:
            if stage >= 1:
                make_hT(x_own, big, B_hT, pb)
            S.barrier()
        if debug and stage >= 1:
            with ExitStack() as pd:
                dt_, B_dt = sb("dbgt", [128, 16, 512], F32, pd)
                for s4 in range(4):
                    S.op("dve", lambda e: e.tensor_copy(out=dt_[:], in_=big[:, :, s4 * 512:(s4 + 1) * 512]), [B_hT], [B_dt])
                    S.dma("sp", dbg["d_h"][:, :, s4 * 512:(s4 + 1) * 512], dt_[:], [B_dt], [])
                S.barrier()

        if stage >= 2:
            with ExitStack() as pdd:
                QT, B_QT = sb("QT", [128, TOK], BF16, pdd)
                KT, B_KT = sb("KT", [128, TOK], BF16, pdd)
                VB, B_VB = sb("VB", [128, 16, 128], BF16, pdd)
                ND, B_NUM = sb("ND", [128, 2, TOK], F32, pdd)
                B_DEN = B_NUM
                NUM = ND[:, 0, :]
                DEN = ND[:, 1, :]
                OA, B_OA = sb("OA", [128, TOK], BF16, pdd)
                ebt, B_ebt = sb("ebt", [128, 256], F32, pdd)
                ebf, B_ebf = sb("ebf", [128, 256], F32, pdd)
                esb = [sb(f"esb{i}", [128, 256], F32, pdd) for i in range(2)]
                Pt = [sb(f"Pt{i}", [128, 256], BF16, pdd) for i in range(2)]
                cpi = 0
                for j in range(4):
                    for g in range(3):
                        d = DIL[g]
                        span = 128 * d
                        hh = g * 4 + j
                        S.dma("sp", ebt[:], alibi_d[hh], [], [B_ebt])
                        S.op("dve", lambda e: e.tensor_copy(out=ebf[:, 128:256], in_=ebt[:, 128:256]), [B_ebt], [B_ebf])
                        S.op("dve", lambda e: e.tensor_scalar(out=ebf[:, 0:128], in0=ebt[:, 0:128], scalar1=flag[:, 0:1],
                                                              scalar2=None, op0=ALU.mult), [B_ebt, B_flag], [B_ebf])
                        wq = load_wchunk(COL_QA + hh * 128)
                        wk = load_wchunk(COL_KA + hh * 128)
                        wv = load_wchunk(COL_VA + hh * 128)
                        for s4 in range(4):
                            pi = cpi % 2
                            cpi += 1
                            inproj_fm(wq, big, B_hT, s4 * 512, 512, ps[pi], psb[pi])
                            S.op("act", lambda e: e.copy(out=QT[:, s4 * 512:(s4 + 1) * 512], in_=ps[pi][:]), [psb[pi]], [B_QT])
                            pi = cpi % 2
                            cpi += 1
                            inproj_fm(wk, big, B_hT, s4 * 512, 512, ps[pi], psb[pi])
                            S.op("dve", lambda e: e.tensor_copy(out=KT[:, s4 * 512:(s4 + 1) * 512], in_=ps[pi][:]), [psb[pi]], [B_KT])
                        for b0 in range(0, 16, 4):
                            pi = 2 + (cpi % 2)
                            cpi += 1
                            pv = ps[pi][:].rearrange("p (k t) -> p k t", t=128)
                            for bb in range(4):
                                blk = b0 + bb
                                nsp, r = blk // d, blk % d
                                tsl = slice(nsp * span + r, (nsp + 1) * span, d)
                                S.mm(pv[:, bb, :], [(big[:, kc, tsl], wv[0][:, kc, :]) for kc in range(16)], [wv[1], B_hT],
                                     [psb[pi]], ticket=True)
                            S.op("act", lambda e: e.copy(out=VB[:, b0:b0 + 4, :], in_=pv), [psb[pi]], [B_VB])
                        DSTOP = int(os.environ.get("DSTOP", "9"))
                        for blk in (range(16) if DSTOP >= 2 else []):
                            nsp, r = blk // d, blk % d
                            qsl = slice(nsp * span + r, (nsp + 1) * span, d)
                            if nsp == 0:
                                kprev = KH[g][:, j, r:span:d]
                                vprev = VH[g][:, j, r, :]
                                rdk = [B_KH[g]]
                                rdv = [B_VH[g]]
                                tab, B_tab = ebf, B_ebf
                            else:
                                kprev = KT[:, (nsp - 1) * span + r:nsp * span:d]
                                vprev = VB[:, (nsp - 1) * d + r, :]
                                rdk = []
                                rdv = []
                                tab, B_tab = ebt, B_ebt
                            bi = blk % 2
                            pS, B_pS = ps[4 + bi], psb[4 + bi]
                            pO, B_pO = ps[6 + bi], psb[6 + bi]
                            S.mm(pS[:, 0:128], [(kprev, QT[:, qsl])], [B_QT, B_KT] + rdk, [B_pS], ticket=True)
                            S.mm(pS[:, 128:256], [(KT[:, qsl], QT[:, qsl])], [B_QT, B_KT] + rdk, [B_pS])
                            et, B_et = esb[bi]
                            pt, B_pt = Pt[bi]
                            S.op("act", lambda e: e.activation(out=et[:], in_=pS[:, 0:256], func=AF.Exp, scale=128 ** -0.5),
                                 [B_pS], [B_et])
                            S.op("dve", lambda e: e.tensor_tensor(out=pt[:], in0=et[:], in1=tab[:], op=ALU.mult),
                                 [B_et, B_tab], [B_pt])
                            if DSTOP < 3:
                                continue
                            S.mm(pO[:, 0:128], [(vprev, pt[:, 0:128]), (VB[:, blk, :], pt[:, 128:256])], [B_pt, B_VB] + rdv,
                                 [B_pO], ticket=True)
                            S.mm(pO[:, 128:256], [(ones_bf[:], pt[:, 0:128]), (ones_bf[:], pt[:, 128:256])], [B_pt, B_ones], [B_pO])
                            if DSTOP < 4:
                                continue
                            pOv = pO[:, 0:256].rearrange("p (a q) -> p a q", a=2)
                            if g == 0:
                                S.op("dve", lambda e: e.tensor_copy(out=ND[:, :, qsl], in_=pOv), [B_pO], [B_NUM])
                            else:
                                S.op("dve", lambda e: e.tensor_tensor(out=ND[:, :, qsl], in0=ND[:, :, qsl], in1=pOv, op=ALU.add),
                                     [B_pO, B_NUM], [B_NUM])
                    S.op("dve", lambda e: e.reciprocal(out=DEN, in_=DEN), [B_DEN], [B_DEN])
                    S.op("dve", lambda e: e.tensor_tensor(out=OA[:], in0=NUM, in1=DEN, op=ALU.mult), [B_NUM], [B_OA])
                    S.dma("sp", oa_scr[:, j, :], OA[:], [B_OA], [B_oascr[j]])
                S.barrier()

        if stage >= 3:
            with ExitStack() as pe_:
                tmps = [hgrn_tmp(pe_), hgrn_tmp(pe_)]
                Sbf = [sb(f"Sbf{i}", [128, 128], BF16, pe_) for i in range(8)]
                orts = [sb(f"ORT{i}", [128, TOK], BF16, pe_) for i in range(2)]
                units = [(hh, seg) for hh in range(8) for seg in range(4)]
                wts_h = {}
                curbox = [0]

                def drain(g):
                    for _ in g:
                        pass

                for ui in range(len(units) + 1):
                    fr = None
                    bk = None
                    if ui < len(units):
                        hh, seg = units[ui]
                        if seg == 0:
                            wts_h[hh] = (load_wchunk(COL_QR + hh * 128), load_wchunk(COL_FR + hh * 128),
                                         load_wchunk(COL_IR + hh * 128), load_wchunk(COL_GR + hh * 128))
                        fr = hgrn_front(hh, big, seg * 512, True, wts_h[hh], tmps[ui % 2])
                    if ui > 0:
                        ph_, ps_ = units[ui - 1]
                        ORT, B_ORT = orts[ph_ % 2]
                        if ps_ == 0:
                            S.op("act", lambda e: e.copy(out=Sbf[0][0][:], in_=SH[:, ph_, :]), [B_SH], [Sbf[0][1]])
                            curbox[0] = 0
                        bk = hgrn_back(ph_, ps_ * 512, True, tmps[(ui - 1) % 2], SH[:, ph_, :], B_SH, Sbf, curbox, ORT, B_ORT)
                    if fr is not None:
                        drain(fr)
                    if bk is not None:
                        drain(bk)
                    if ui > 0 and ps_ == 3:
                        S.dma("sp", oa_scr[:, 4 + ph_, :], ORT[:], [B_ORT], [B_oascr[4 + ph_]])
                S.barrier()
        if debug and stage >= 2:
            with ExitStack() as pd:
                db_, B_db = sb("dbgb", [128, 12, 512], BF16, pd)
                dt_, B_dt = sb("dbgt", [128, 12, 512], F32, pd)
                for s4 in range(4):
                    S.dma("sp", db_[:], oa_scr[:, :, s4 * 512:(s4 + 1) * 512], B_oascr, [B_db])
                    S.op("dve", lambda e: e.tensor_copy(out=dt_[:], in_=db_[:]), [B_db], [B_dt])
                    S.dma("sp", dbg["d_oa"][:, :, s4 * 512:(s4 + 1) * 512], dt_[:], [B_dt], [])
                S.barrier()

        hp.close()
        if stage >= 4:
            with ExitStack() as pf:
                OAall, B_OAall = sb("OAall", [128, 12, TOK], BF16, pf)
                wbr = [sb(f"wbr{i}", [128, 12, 128], BF16, pf) for i in range(2)]
                sg = [sb(f"sg{i}", [128, 512], F32, pf) for i in range(2)]
                m1, B_m1 = sb("m1", [128, 512], F32, pf)
                m2, B_m2 = sb("m2", [128, 512], F32, pf)
                mtc = [sb(f"mtc{i}", [128, TOK], BF16, pf) for i in range(2)]
                S.dma("sp", OAall[:], oa_scr, B_oascr, [B_OAall])
                for c in range(16):
                    wga = load_wchunk(COL_GA + c * 128)
                    wgb = load_wchunk(COL_GB + c * 128)
                    wb_t, B_wb = wbr[c % 2]
                    S.dma("pool", wb_t[:, 0:4, :], w_ba[c], [], [B_wb])
                    S.dma("pool", wb_t[:, 4:12, :], w_bh[c], [], [B_wb])
                    MTc, B_MTc = mtc[c % 2]
                    for s4 in range(4):
                        t0 = s4 * 512
                        inproj_fm(wga, big, B_hT, t0, 512, ps[0], psb[0])
                        S.op("act", lambda e: e.activation(out=sg[0][0][:], in_=ps[0][:], func=AF.Sigmoid), [psb[0]], [sg[0][1]])
                        S.mm(ps[1][:], [(wb_t[:, jj, :], OAall[:, jj, t0:t0 + 512]) for jj in range(4)], [B_wb, B_OAall], [psb[1]])
                        S.op("dve", lambda e: e.tensor_tensor(out=m1[:], in0=sg[0][0][:], in1=ps[1][:], op=ALU.mult),
                             [sg[0][1], psb[1]], [B_m1])
                        inproj_fm(wgb, big, B_hT, t0, 512, ps[2], psb[2])
                        S.op("act", lambda e: e.activation(out=sg[1][0][:], in_=ps[2][:], func=AF.Sigmoid), [psb[2]], [sg[1][1]])
                        S.mm(ps[3][:], [(wb_t[:, jj, :], OAall[:, jj, t0:t0 + 512]) for jj in range(4, 12)], [B_wb, B_OAall], [psb[3]])
                        S.op("dve", lambda e: e.tensor_tensor(out=m2[:], in0=sg[1][0][:], in1=ps[3][:], op=ALU.mult),
                             [sg[1][1], psb[3]], [B_m2])
                        S.op("dve", lambda e: e.tensor_tensor(out=MTc[:, t0:t0 + 512], in0=m1[:], in1=m2[:], op=ALU.add),
                             [B_m1, B_m2], [B_MTc])
                    S.dma("sp", mt_scr.rearrange("t p c k -> p c t k")[:, c], MTc[:].rearrange("p (t k) -> p t k", k=128),
                          [B_MTc], [B_mtscr])
                S.barrier()
        mid.close()
        if stage >= 4:
            with ExitStack() as pf:
                Wo, B_Wo = sb("Wo", [128, 16, D], BF16, pf)
                GMt, B_GMt = sb("GMt", [128, D], F32, pf)
                G2t, B_G2t = sb("G2t", [128, D], F32, pf)
                SH2t, B_SH2t = sb("SH2t", [128, D], F32, pf)
                mtt = [sb(f"mtt{i}", [128, 16, 128], BF16, pf) for i in range(2)]
                xr = [sb(f"xf{i}", [128, D], F32, pf) for i in range(2)]
                tmpf_r = [sb(f"tmpf{i}", [128, D], F32, pf) for i in range(2)]
                h2b_r = [sb(f"h2b{i}", [128, D], BF16, pf) for i in range(2)]
                h2t_r = [sb(f"h2tt{i}", [128, 16, 128], BF16, pf) for i in range(2)]
                junk_r = [sb(f"junkf{i}", [128, D], BF16, pf) for i in range(2)]
                ss_r = [sb(f"ssf{i}", [128, 4], F32, pf) for i in range(2)]
                m1_r = [sb(f"m1s{i}", [128, 8], F32, pf) for i in range(2)]
                yb_r = [sb(f"ybuf{i}", [128, D], F32, pf) for i in range(2)]
                yb2_b = [Buf("yb2a"), Buf("yb2b")]
                wrt, B_wrt = sb("wrt", [128, 16, NE], BF16, pf)
                rt = {k: sb("rt_" + k, shp, F32, pf) for k, shp in
                      (("sc", [128, 64]), ("ch", [128, 64]), ("m1", [128, 8]), ("eq", [128, 64]), ("ch2", [128, 64]),
                       ("m2", [128, 8]), ("gs", [128, 8]), ("t8", [128, 8]), ("gm", [128, 8]), ("chm", [128, 64]),
                       ("t8e", [128, 8]), ("sel", [128, 64]), ("gsel", [128, 64]), ("den", [128, 2]))}
                S.dma("pool", wrt[:], w_r, [], [B_wrt])
                for pc in range(8):
                    S.dma("pool", Wo[:, :, pc * 256:(pc + 1) * 256], w_out[pc], [], [B_Wo])
                S.dma("sp", GMt[:], modrep[2], [B_modrep[2]], [B_GMt])
                S.dma("sp", G2t[:], modrep[4], [B_modrep[4]], [B_G2t])
                S.dma("sp", SH2t[:], modrep[3], [B_modrep[3]], [B_SH2t])
                def f2_outproj(tile_i):
                    MTt, B_MTt = mtt[tile_i % 2]
                    S.dma("sp", MTt[:], mt_scr[tile_i], [B_mtscr], [B_MTt])
                    for n in range(4):
                        S.mm(ps[4 + n][:], [(MTt[:, kc, :], Wo[:, kc, n * 512:(n + 1) * 512]) for kc in range(16)],
                             [B_MTt, B_Wo], [psb[4 + n]])

                f2_outproj(0)
                for tile_i in range(16):
                    r0 = tile_i * 128
                    tmpf, B_tmpf = tmpf_r[tile_i % 2]
                    h2b, B_h2b = h2b_r[tile_i % 2]
                    h2t, B_h2tt = h2t_r[tile_i % 2]
                    junk, B_junk = junk_r[tile_i % 2]
                    ss, B_ss = ss_r[tile_i % 2]
                    m1, B_m1 = m1_r[tile_i % 2]
                    xin, B_x = xr[tile_i % 2]
                    S.dma("sp", xin[:], x_own[r0:r0 + 128, :], [], [B_x])
                    yb, B_yb = yb_r[tile_i % 2]
                    B_yb2 = yb2_b[tile_i % 2]
                    for n in range(4):
                        nsl = slice(n * 512, (n + 1) * 512)
                        if n % 2 == 0:
                            S.op("act", lambda e: e.copy(out=yb[:, nsl], in_=ps[4 + n][:]), [psb[4 + n]], [B_yb])
                        else:
                            S.op("dve", lambda e: e.tensor_copy(out=yb[:, nsl], in_=ps[4 + n][:]), [psb[4 + n]], [B_yb2])
                    S.op("dve", lambda e: e.memset(ss[:, 0:1], 0.0), [], [B_ss])
                    S.op("act", lambda e: e.activation(out=junk[:], in_=yb[:], func=AF.Square, accum_out=ss[:, 0:1]),
                         [B_yb, B_yb2], [B_junk, B_ss])
                    S.op("dve", lambda e: e.tensor_scalar(out=ss[:, 1:2], in0=ss[:, 0:1], scalar1=1.0 / D, scalar2=EPS,
                                                          op0=ALU.mult, op1=ALU.add), [B_ss], [B_ss])
                    S.op("act", lambda e: e.activation(out=ss[:, 2:3], in_=ss[:, 1:2], func=AF.Sqrt), [B_ss], [B_ss])
                    S.op("dve", lambda e: e.reciprocal(out=ss[:, 3:4], in_=ss[:, 2:3]), [B_ss], [B_ss])
                    S.op("dve", lambda e: e.scalar_tensor_tensor(out=tmpf[:], in0=yb[:], scalar=ss[:, 3:4], in1=GMt[:],
                                                                 op0=ALU.mult, op1=ALU.mult), [B_yb, B_yb2, B_ss, B_GMt], [B_tmpf])
                    S.op("dve", lambda e: e.tensor_tensor(out=xin[:], in0=xin[:], in1=tmpf[:], op=ALU.add), [B_x, B_tmpf], [B_x])
                    S.dma("pool", out_d[r0:r0 + 128, :], xin[:], [B_x], [B_out[tile_i]])
                    if debug:
                        S.dma("sp", dbg["d_x1"][r0:r0 + 128, :], xin[:], [B_x], [])
                    if tile_i < 15:
                        f2_outproj(tile_i + 1)
                    rstd = rms_rstd(xin[:], B_x, junk[:], B_junk, ss, B_ss, D)
                    S.op("dve", lambda e: e.scalar_tensor_tensor(out=tmpf[:], in0=xin[:], scalar=rstd, in1=G2t[:],
                                                                 op0=ALU.mult, op1=ALU.mult), [B_x, B_ss, B_G2t], [B_tmpf])
                    S.op("dve", lambda e: e.tensor_tensor(out=h2b[:], in0=tmpf[:], in1=SH2t[:], op=ALU.add), [B_tmpf, B_SH2t], [B_h2b])
                    for half in range(2):
                        pi = 2 + half
                        pv = ps[pi][:].bitcast(BF16).rearrange("p (k t) -> p k t", t=128)
                        S.deps("pe", [B_h2b, B_ident], [psb[pi]])
                        ins = None
                        for k in range(8):
                            kc = half * 8 + k
                            ins = nc.tensor.transpose(pv[:, k, :], h2b[:, kc * 128:(kc + 1) * 128], ident_bf[:])
                        S.pe_ticket_after(ins, [B_h2b, B_ident], [psb[pi]])
                        S.op("act", lambda e: e.copy(out=h2t[:, half * 8:half * 8 + 8, :], in_=pv), [psb[pi]], [B_h2tt])
                    S.dma("pool", h2t_scr[:, :, r0:r0 + 128], h2t[:], [B_h2tt], [B_h2t[tile_i]])
                    S.mm(ps[0][:, 0:NE], [(h2t[:, kc, :], wrt[:, kc, :]) for kc in range(16)], [B_h2tt, B_wrt], [psb[0]])
                    R = lambda k: rt[k][0]
                    RB = lambda k: rt[k][1]
                    v3 = lambda a: a.rearrange("p (g k) -> p g k", k=8)
                    S.op("act", lambda e: e.activation(out=R("sc")[:], in_=ps[0][:, 0:NE], func=AF.Sigmoid), [psb[0]], [RB("sc")])
                    S.op("dve", lambda e: e.tensor_tensor(out=R("ch")[:], in0=R("sc")[:], in1=rbias[:], op=ALU.add),
                         [RB("sc"), B_rbias], [RB("ch")])
                    S.op("dve", lambda e: e.tensor_reduce(out=R("m1")[:], in_=v3(R("ch")[:]), axis=AX.X, op=ALU.max),
                         [RB("ch")], [RB("m1")])
                    S.op("dve", lambda e: e.tensor_tensor(out=v3(R("eq")[:]), in0=v3(R("ch")[:]),
                                                          in1=R("m1")[:].unsqueeze(2).to_broadcast([128, 8, 8]), op=ALU.is_equal),
                         [RB("ch"), RB("m1")], [RB("eq")])
                    S.op("dve", lambda e: e.scalar_tensor_tensor(out=R("ch2")[:], in0=R("eq")[:], scalar=-1e9, in1=R("ch")[:],
                                                                 op0=ALU.mult, op1=ALU.add), [RB("eq"), RB("ch")], [RB("ch2")])
                    S.op("dve", lambda e: e.tensor_reduce(out=R("m2")[:], in_=v3(R("ch2")[:]), axis=AX.X, op=ALU.max),
                         [RB("ch2")], [RB("m2")])
                    S.op("dve", lambda e: e.tensor_tensor(out=R("gs")[:], in0=R("m1")[:], in1=R("m2")[:], op=ALU.add),
                         [RB("m1"), RB("m2")], [RB("gs")])
                    S.op("dve", lambda e: e.max(out=R("t8")[:], in_=R("gs")[:]), [RB("gs")], [RB("t8")])
                    S.op("dve", lambda e: e.tensor_scalar(out=R("gm")[:], in0=R("gs")[:], scalar1=R("t8")[:, 3:4], scalar2=None,
                                                          op0=ALU.is_ge), [RB("gs"), RB("t8")], [RB("gm")])
                    S.op("dve", lambda e: e.tensor_scalar(out=R("gm")[:], in0=R("gm")[:], scalar1=1e9, scalar2=-1e9,
                                                          op0=ALU.mult, op1=ALU.add), [RB("gm")], [RB("gm")])
                    S.op("dve", lambda e: e.tensor_tensor(out=v3(R("chm")[:]), in0=v3(R("ch")[:]),
                                                          in1=R("gm")[:].unsqueeze(2).to_broadcast([128, 8, 8]), op=ALU.add),
                         [RB("ch"), RB("gm")], [RB("chm")])
                    S.op("dve", lambda e: e.max(out=R("t8e")[:], in_=R("chm")[:]), [RB("chm")], [RB("t8e")])
                    S.op("dve", lambda e: e.tensor_scalar(out=R("sel")[:], in0=R("chm")[:], scalar1=R("t8e")[:, 7:8], scalar2=None,
                                                          op0=ALU.is_ge), [RB("chm"), RB("t8e")], [RB("sel")])
                    S.op("dve", lambda e: e.tensor_tensor(out=R("gsel")[:], in0=R("sel")[:], in1=R("sc")[:], op=ALU.mult),
                         [RB("sel"), RB("sc")], [RB("gsel")])
                    S.op("dve", lambda e: e.tensor_reduce(out=R("den")[:, 0:1], in_=R("gsel")[:], axis=AX.X, op=ALU.add),
                         [RB("gsel")], [RB("den")])
                    S.op("dve", lambda e: e.reciprocal(out=R("den")[:, 1:2], in_=R("den")[:, 0:1]), [RB("den")], [RB("den")])
                    S.op("dve", lambda e: e.tensor_scalar(out=Gt[:, tile_i, 0:NE], in0=R("gsel")[:], scalar1=R("den")[:, 1:2],
                                                          scalar2=2.5, op0=ALU.mult, op1=ALU.mult), [RB("gsel"), RB("den")], [B_Gt])
                if debug:
                    S.dma("sp", dbg["d_g"], Gt[:], [B_Gt], [])
                S.barrier()
        mid.close()
        if stage >= 5:
            with ExitStack() as pg:
                h2p, B_h2p = sb("h2p", [128, 16, 1024], BF16, pg)
                yacc, B_yacc = sb("yacc", [128, 8, D], F32, pg)
                ss, B_ss = sb("ssg", [128, 4], F32, pg)
                for p in range(2):
                    S.dma("sp", h2p[:], h2t_scr[:, :, p * 1024:(p + 1) * 1024], B_h2t, [B_h2p])
                    S.op("pool", lambda e: e.memset(yacc[:], 0.0), [], [B_yacc])
                    with ExitStack() as pw:
                        xgu = [(sb(f"xg{i}", [128, 16, 256], BF16, pw), sb(f"xu{i}", [128, 16, 256], BF16, pw)) for i in range(3)]
                        xdr = [sb(f"xd{i}", [128, 4, D], BF16, pw) for i in range(2)]
                        hid = [[sb(f"hid{a}{t}", [128, 4, 512], BF16, pw) for t in range(2)] for a in range(2)]
                        sgl = [sb(f"sgl{i}", [128, 512], F32, pw) for i in range(2)]
                        di = [0]

                        def emit_down(ex, regions):
                            xd, B_xd = xdr[ex % 2]
                            for (tg, i, n) in regions:
                                hd_, B_hd = hid[ex % 2][tg]
                                lt = tg * 4 + i
                                pi = 4 + (di[0] % 4)
                                di[0] += 1
                                nsl = slice(n * 512, (n + 1) * 512)
                                S.mm(ps[pi][:], [(hd_[:, kc, i * 128:(i + 1) * 128], xd[:, kc, nsl]) for kc in range(4)],
                                     [B_hd, B_xd], [psb[pi]])
                                S.op("dve", lambda e: e.scalar_tensor_tensor(out=yacc[:, lt, nsl], in0=ps[pi][:],
                                                                             scalar=Gt[:, p * 8 + lt, ex:ex + 1], in1=yacc[:, lt, nsl],
                                                                             op0=ALU.mult, op1=ALU.add),
                                     [psb[pi], B_Gt, B_yacc], [B_yacc])

                        allreg = [(tg, i, n) for tg in range(2) for i in range(4) for n in range(4)]
                        for ex in range(NE + 1):
                            gsrc = w_ge[ex] if ex < NE else w_gs
                            usrc = w_ue[ex] if ex < NE else w_us
                            dsrc = w_de[ex] if ex < NE else w_ds
                            slots = []
                            for hf in range(2):
                                (xg, B_xg), (xu, B_xu) = xgu[(2 * ex + hf) % 3]
                                hsl = slice(hf * 256, (hf + 1) * 256)
                                S.dma("pool", xg[:], gsrc[hf], [], [B_xg])
                                S.dma("pool", xu[:], usrc[hf], [], [B_xu])
                                slots.append(((xg, B_xg), (xu, B_xu)))
                            xd, B_xd = xdr[ex % 2]
                            S.dma("pool", xd[:], dsrc, [], [B_xd])
                            u = 0
                            for hf in range(2):
                                (xg, B_xg), (xu, B_xu) = slots[hf]
                                for tg in range(2):
                                    tsl = slice(tg * 512, (tg + 1) * 512)
                                    hd_, B_hd = hid[ex % 2][tg]
                                    for ch in range(2):
                                        csl = slice(ch * 128, (ch + 1) * 128)
                                        S.mm(ps[ch][:], [(xg[:, kc, csl], h2p[:, kc, tsl]) for kc in range(16)], [B_xg, B_h2p], [psb[ch]])
                                        S.mm(ps[2 + ch][:], [(xu[:, kc, csl], h2p[:, kc, tsl]) for kc in range(16)], [B_xu, B_h2p], [psb[2 + ch]])
                                    for ch in range(2):
                                        sg_, B_sg = sgl[ch]
                                        S.op("act", lambda e: e.activation(out=sg_[:], in_=ps[ch][:], func=AF.Silu), [psb[ch]], [B_sg])
                                        S.op("dve", lambda e: e.tensor_tensor(out=hd_[:, hf * 2 + ch, :], in0=sg_[:], in1=ps[2 + ch][:], op=ALU.mult),
                                             [B_sg, psb[2 + ch]], [B_hd])
                                    if ex > 0:
                                        emit_down(ex - 1, allreg[u * 8:(u + 1) * 8])
                                    u += 1
                        emit_down(NE, allreg)
                        S.barrier()
                    with ExitStack() as pz:
                        xin_r = [sb(f"xfin{i}", [128, D], F32, pz) for i in range(3)]
                        mrep, B_mrep = sb("mrepg", [128, D], F32, pz)
                        junk_r = [sb(f"junkg{i}", [128, D], BF16, pz) for i in range(2)]
                        ss_r = [sb(f"ssg{i}", [128, 4], F32, pz) for i in range(2)]
                        S.dma("sp", mrep[:], modrep[5], [B_modrep[5]], [B_mrep])
                        B_yt = [Buf(f"yacc_t{p}_{i}") for i in range(8)]
                        for lt in range(8):
                            r0 = (p * 8 + lt) * 128
                            B_yacc_l = B_yt[lt]
                            xin, B_x = xin_r[lt % 3]
                            junk, B_junk = junk_r[lt % 2]
                            ss, B_ss = ss_r[lt % 2]
                            S.dma("pool", xin[:], out_d[r0:r0 + 128, :], [B_out[p * 8 + lt]], [B_x])
                            rstd = rms_rstd(yacc[:, lt, :], B_yacc_l, junk[:], B_junk, ss, B_ss, D)
                            S.op("dve", lambda e: e.scalar_tensor_tensor(out=yacc[:, lt, :], in0=yacc[:, lt, :], scalar=rstd, in1=mrep[:],
                                                                         op0=ALU.mult, op1=ALU.add if False else ALU.mult),
                                 [B_yacc_l, B_ss, B_mrep], [B_yacc_l])
                            S.op("dve", lambda e: e.tensor_tensor(out=xin[:], in0=xin[:], in1=yacc[:, lt, :], op=ALU.add),
                                 [B_x, B_yacc_l], [B_x])
                            S.dma("sp", out_d[r0:r0 + 128, :], xin[:], [B_x], [B_out[p * 8 + lt]])
                        S.barrier()
        S.barrier()
    return nc


def _consts():
    ident = np.eye(128, dtype=np.float32)
    s = np.arange(128)[:, None]
    t = np.arange(128)[None, :]
    tri = ((s // 32 == t // 32) & (s <= t)).astype(np.float32)
    seg = np.ones((128, 512), np.float32)
    seg[:, ::32] = 0.0
    slopes = np.exp2(-8.0 * np.arange(1, 13, dtype=np.float64) / 12)
    alibi = np.zeros((12, 128, 256), np.float32)
    k = np.arange(128)[:, None].astype(np.float64)
    q = np.arange(128)[None, :].astype(np.float64)
    for h in range(12):
        d = DIL[h // 4]
        prev = np.where(k >= q, np.exp(-slopes[h] * d * (q + 128 - k)), 0.0)
        cur = np.where(k <= q, np.exp(-slopes[h] * d * (q - k)), 0.0)
        alibi[h, :, 0:128] = prev
        alibi[h, :, 128:256] = cur
    rowm = (np.arange(128)[:, None] // 32 == np.arange(4)[None, :]).astype(np.float32)
    return ident, tri, seg, alibi, rowm


def make_in_maps(inputs, cores, ned=NE):
    f = lambda a: np.ascontiguousarray(np.asarray(a, dtype=np.float32))
    x = f(inputs["x"])
    c = f(inputs["c"])
    ident, tri, seg, alibi, rowm = _consts()
    norms = np.stack([f(inputs["pre_norm_mix"])[0], f(inputs["post_norm_mix"])[0],
                      f(inputs["pre_norm_ffn"])[0], f(inputs["post_norm_ffn"])[0]], 0)
    lbl = f(inputs["hgrn_lb_logits"]).reshape(2, 8, 128).transpose(2, 0, 1).reshape(128, 16)
    hn = f(inputs["hgrn_norm"])[0].reshape(8, 128).T
    def kmaj(w, cw):
        K, C = w.shape
        return f(np.asarray(w).reshape(K // 128, 128, C // cw, cw).transpose(2, 1, 0, 3))

    def kmaj_e(w, cw):
        E, K, C = w.shape
        return f(np.asarray(w).reshape(E, K // 128, 128, C // cw, cw).transpose(0, 3, 2, 1, 4))

    wde = np.asarray(inputs["w_down_e"][0, :ned])
    shared = {
        "w_ada": kmaj(inputs["w_ada"][0], 512), "b_ada": f(inputs["b_ada"]), "norms": f(norms), "w_in": kmaj(inputs["w_in"][0], 128),
        "lbl": f(lbl), "hn": f(hn), "w_ba": kmaj(inputs["w_branch_attn"][0], 128), "w_bh": kmaj(inputs["w_branch_hgrn"][0], 128),
        "w_out": kmaj(inputs["w_out"][0], 256), "w_r": kmaj(inputs["w_router"][0], NE)[0], "rb": f(inputs["router_bias"]),
        "w_ge": kmaj_e(inputs["w_gate_e"][0, :ned], 256), "w_ue": kmaj_e(inputs["w_up_e"][0, :ned], 256),
        "w_de": f(wde.reshape(wde.shape[0], 4, 128, D).transpose(0, 2, 1, 3)),
        "w_gs": kmaj(inputs["w_gate_s"][0], 256), "w_us": kmaj(inputs["w_up_s"][0], 256),
        "w_ds": f(np.asarray(inputs["w_down_s"][0]).reshape(4, 128, D).transpose(1, 0, 2)),
        "ident": ident, "tri32": tri, "segmask": seg, "alibi": alibi, "rowm": rowm,
    }
    maps = []
    for core in cores:
        b, half = core // 2, core % 2
        m = dict(shared)
        m["x_own"] = f(x[b, half * TOK:(half + 1) * TOK])
        m["x_halo"] = f(x[b, 0:TOK]) if half == 1 else np.zeros((TOK, D), np.float32)
        m["flag"] = np.full((128, 1), float(half), np.float32)
        m["cb"] = f(c[b].reshape(16, 128).T)
        maps.append(m)
    return maps


def kernel(**inputs):
    nc = build()
    cores = list(range(8))
    maps = make_in_maps(inputs, cores)
    res = run_bass_kernel_spmd(nc, maps, core_ids=cores)
    out = np.zeros((4, 4096, D), np.float32)
    for core in cores:
        b, half = core // 2, core % 2
        out[b, half * TOK:(half + 1) * TOK] = res.results[core]["out"]
    return out
```

```python
import os
import numpy as np
from contextlib import ExitStack
import concourse.bass as bass
import concourse.mybir as mybir
from concourse.bass_utils import run_bass_kernel_spmd

F32 = mybir.dt.float32
BF16 = mybir.dt.bfloat16
AF = mybir.ActivationFunctionType
ALU = mybir.AluOpType
AX = mybir.AxisListType

D = 2048
TOK = 2048
EPS = 1e-6
DIL = (1, 4, 16)
NE = 64
COL_QA, COL_KA, COL_VA = 0, 1536, 3072
COL_QR, COL_FR, COL_IR, COL_GR = 4608, 5632, 6656, 7680
COL_GA, COL_GB = 8704, 10752


class Buf:
    __slots__ = ("name", "w", "r")

    def __init__(self, name):
        self.name = name
        self.w = None
        self.r = {}


class Sched:
    ND = 8

    def __init__(self, nc, es):
        self.nc = nc
        self.engs = {"pe": nc.tensor, "act": nc.scalar, "dve": nc.vector, "pool": nc.gpsimd, "sp": nc.sync}
        self.sems = {}
        self.ccnt = {}
        for e in ("pe", "act", "dve", "pool"):
            self.sems["c_" + e] = es.enter_context(nc.semaphore("c_" + e))
            self.ccnt[e] = 0
        self.dcnt = {}
        self.drr = {}
        for q in ("sp", "pool"):
            for i in range(self.ND):
                self.sems[f"d_{q}{i}"] = es.enter_context(nc.semaphore(f"d_{q}{i}"))
                self.dcnt[(q, i)] = 0
            self.drr[q] = 0
        self.waited = {}

    def _wait(self, eng, t):
        if t is None:
            return
        sid, val = t
        if eng == "pe" and sid == "c_pe":
            return
        key = (eng, sid)
        if self.waited.get(key, 0) >= val:
            return
        self.engs[eng].wait_ge(self.sems[sid], val)
        self.waited[key] = val

    def deps(self, eng, reads, writes):
        for b in reads:
            self._wait(eng, b.w)
        for b in writes:
            self._wait(eng, b.w)
            for sid, val in b.r.items():
                self._wait(eng, (sid, val))

    def commit(self, t, reads, writes):
        for b in reads:
            if b.r.get(t[0], 0) < t[1]:
                b.r[t[0]] = t[1]
        for b in writes:
            b.w = t
            b.r = {}

    def op(self, eng, fn, reads=(), writes=()):
        self.deps(eng, reads, writes)
        ins = fn(self.engs[eng])
        self.ccnt[eng] += 1
        ins.then_inc(self.sems["c_" + eng], 1)
        self.commit(("c_" + eng, self.ccnt[eng]), reads, writes)

    def mm(self, out, pairs, reads, writes, start=True, stop=True, ticket=True, **kw):
        self.deps("pe", reads, writes)
        n = len(pairs)
        ins = None
        for i, (l, r) in enumerate(pairs):
            ins = self.nc.tensor.matmul(out, l, r, start=(start and i == 0), stop=(stop and i == n - 1), **kw)
        if ticket:
            self.ccnt["pe"] += 1
            ins.then_inc(self.sems["c_pe"], 1)
            self.commit(("c_pe", self.ccnt["pe"]), reads, writes)

    def pe_ticket_after(self, ins, reads, writes):
        self.ccnt["pe"] += 1
        ins.then_inc(self.sems["c_pe"], 1)
        self.commit(("c_pe", self.ccnt["pe"]), reads, writes)

    def dma(self, q, out, in_, reads, writes):
        i = self.drr[q]
        self.drr[q] = (i + 1) % self.ND
        sid = f"d_{q}{i}"
        prev = self.dcnt[(q, i)]
        if prev > 0:
            self._wait(q, (sid, 16 * prev))
        self.deps(q, reads, writes)
        self.engs[q].dma_start(out=out, in_=in_).then_inc(self.sems[sid], 16)
        self.dcnt[(q, i)] += 1
        self.commit((sid, 16 * self.dcnt[(q, i)]), reads, writes)

    def barrier(self):
        tickets = [("c_" + e, self.ccnt[e]) for e in ("pe", "act", "dve", "pool") if self.ccnt[e] > 0]
        for (q, i), c in self.dcnt.items():
            if c > 0:
                tickets.append((f"d_{q}{i}", 16 * c))
        for e in ("pe", "act", "dve", "pool", "sp"):
            for t in tickets:
                self._wait(e, t)


def build(stage=99, debug=False):
    nc = bass.Bass("TRN2", target_bir_lowering=False)

    def din(name, shape, dt=F32):
        return nc.dram_tensor(name, list(shape), dt, kind="ExternalInput").ap()

    x_own = din("x_own", [TOK, D])
    x_halo = din("x_halo", [TOK, D])
    flag_d = din("flag", [128, 1])
    cb_d = din("cb", [128, 16])
    w_ada = din("w_ada", [24, 128, 16, 512])
    b_ada = din("b_ada", [1, 6 * D])
    norms_d = din("norms", [4, D])
    w_in = din("w_in", [100, 128, 16, 128])
    lbl_d = din("lbl", [128, 16])
    hn_d = din("hn", [128, 8])
    w_ba = din("w_ba", [16, 128, 4, 128])
    w_bh = din("w_bh", [16, 128, 8, 128])
    w_out = din("w_out", [8, 128, 16, 256])
    w_r = din("w_r", [128, 16, NE])
    rb_d = din("rb", [1, NE])
    NED = NE if stage >= 5 else 1
    w_ge = din("w_ge", [NED, 2, 128, 16, 256])
    w_ue = din("w_ue", [NED, 2, 128, 16, 256])
    w_de = din("w_de", [NED, 128, 4, D])
    w_gs = din("w_gs", [2, 128, 16, 256])
    w_us = din("w_us", [2, 128, 16, 256])
    w_ds = din("w_ds", [128, 4, D])
    ident_d = din("ident", [128, 128])
    tri_d = din("tri32", [128, 128])
    seg_d = din("segmask", [128, 512])
    alibi_d = din("alibi", [12, 128, 256])
    rowm_d = din("rowm", [128, 4])
    out_d = nc.dram_tensor("out", [TOK, D], F32, kind="ExternalOutput").ap()
    modrep = nc.dram_tensor("modrep", [6, 128, D], F32).ap()
    oa_scr = nc.dram_tensor("oa_scr", [128, 12, TOK], BF16).ap()
    h2t_scr = nc.dram_tensor("h2t_scr", [128, 16, TOK], BF16).ap()
    mt_scr = nc.dram_tensor("mt_scr", [16, 128, 16, 128], BF16).ap()
    dbg = {}
    if debug:
        for nm, shp in (("d_h", [128, 16, TOK]), ("d_oa", [128, 12, TOK]), ("d_x1", [TOK, D]), ("d_g", [128, 16, 65])):
            dbg[nm] = nc.dram_tensor(nm, shp, F32, kind="ExternalOutput").ap()

    B_out = [Buf(f"out{i}") for i in range(16)]
    B_modrep = [Buf(f"modrep{i}") for i in range(6)]
    B_oascr = [Buf(f"oascr{i}") for i in range(12)]
    B_h2t = [Buf(f"h2t{i}") for i in range(16)]
    B_mtscr = Buf("mtscr")

    with ExitStack() as es:
        S = Sched(nc, es)

        uid = [0]

        def sb(name, shape, dt, stack=es):
            uid[0] += 1
            name = f"{name}_{uid[0]}"
            t = stack.enter_context(nc.sbuf_tensor(name, list(shape), dt))
            return t, Buf(name)

        ps = []
        psb = []
        for i in range(8):
            ps.append(es.enter_context(nc.psum_tensor(f"ps{i}", [128, 512], F32)))
            psb.append(Buf(f"ps{i}"))

        ident_bf, B_ident = sb("ident_bf", [128, 128], BF16)
        tri, B_tri = sb("tri", [128, 128], F32)
        segm, B_segm = sb("segm", [128, 512], F32)
        ones_bf, B_ones = sb("ones_bf", [128, 128], BF16)
        flag, B_flag = sb("flag_t", [128, 1], F32)
        lbl, B_lbl = sb("lbl_t", [128, 16], F32)
        lb, B_lb = sb("lb", [128, 8], F32)
        oml, B_oml = sb("oml", [128, 8], F32)
        hn, B_hn = sb("hn_t", [128, 8], F32)
        rbias, B_rbias = sb("rbias", [128, NE], F32)
        Gt, B_Gt = sb("Gt", [128, 16, 65], F32)
        S.dma("pool", ident_bf[:], ident_d, [], [B_ident])
        S.dma("sp", tri[:], tri_d, [], [B_tri])
        rowm, B_rowm = sb("rowm", [128, 4], F32)
        S.dma("sp", rowm[:], rowm_d, [], [B_rowm])
        S.dma("sp", segm[:], seg_d, [], [B_segm])
        S.dma("sp", flag[:], flag_d, [], [B_flag])
        S.dma("sp", lbl[:], lbl_d, [], [B_lbl])
        S.dma("sp", hn[:], hn_d, [], [B_hn])
        S.dma("sp", rbias[:], rb_d[0:1, :].to_broadcast([128, NE]), [], [B_rbias])
        S.op("dve", lambda e: e.memset(ones_bf[:], 1.0), [], [B_ones])
        S.op("dve", lambda e: e.memset(Gt[:], 1.0), [], [B_Gt])
        S.op("dve", lambda e: e.tensor_tensor(out=lb[:], in0=lbl[:, 0:8], in1=lbl[:, 8:16], op=ALU.subtract), [B_lbl], [B_lb])
        S.op("act", lambda e: e.activation(out=lb[:], in_=lb[:], func=AF.Sigmoid), [B_lb], [B_lb])
        S.op("dve", lambda e: e.tensor_scalar(out=oml[:], in0=lb[:], scalar1=-1.0, scalar2=1.0, op0=ALU.mult, op1=ALU.add),
             [B_lb], [B_oml])

        NWR = 6
        mid = ExitStack()
        wring = [sb(f"wring{i}", [128, 16, 128], BF16, mid) for i in range(NWR)]
        big, B_hT = sb("hTbig", [128, 16, TOK], BF16, mid)
        hp = ExitStack()
        wr_i = [0]

        def load_wchunk(col0):
            t, b = wring[wr_i[0] % NWR]
            wr_i[0] += 1
            S.dma("pool", t[:], w_in[col0 // 128], [], [b])
            return t, b

        def inproj_fm(w, hT, B_hT, t0, n, pst, B_ps, off=0):
            wt, wb = w
            S.mm(pst[:, off:off + n], [(wt[:, kc, :], hT[:, kc, t0:t0 + n]) for kc in range(16)], [wb, B_hT], [B_ps])

        def inproj_tm(w, hT, B_hT, tsl, pst_ap, B_ps):
            wt, wb = w
            S.mm(pst_ap, [(hT[:, kc, tsl], wt[:, kc, :]) for kc in range(16)], [wb, B_hT], [B_ps])

        def rms_rstd(src_ap, B_src, junk, B_junk, ss, B_ss, nfeat):
            S.op("dve", lambda e: e.memset(ss[:, 0:1], 0.0), [], [B_ss])
            S.op("act", lambda e: e.activation(out=junk, in_=src_ap, func=AF.Square, accum_out=ss[:, 0:1]),
                 [B_src], [B_junk, B_ss])
            S.op("dve", lambda e: e.tensor_scalar(out=ss[:, 1:2], in0=ss[:, 0:1], scalar1=1.0 / nfeat, scalar2=EPS,
                                                  op0=ALU.mult, op1=ALU.add), [B_ss], [B_ss])
            S.op("act", lambda e: e.activation(out=ss[:, 2:3], in_=ss[:, 1:2], func=AF.Sqrt), [B_ss], [B_ss])
            S.op("dve", lambda e: e.reciprocal(out=ss[:, 3:4], in_=ss[:, 2:3]), [B_ss], [B_ss])
            return ss[:, 3:4]

        with ExitStack() as pa:
          if stage >= -1:
                sc, B_sc = sb("sc", [128, 16], F32, pa)
                screp, B_screp = sb("screp", [128, 16, 128], BF16, pa)
                modt, B_modt = sb("modt", [128, D], F32, pa)
                nrm, B_nrm = sb("nrm", [128, D], F32, pa)
                bad, B_bad = sb("bad", [128, D], F32, pa)
                wad = [sb(f"wad{i}", [128, 16, 512], BF16, pa) for i in range(2)]
                S.dma("sp", sc[:], cb_d, [], [B_sc])
                S.op("act", lambda e: e.activation(out=sc[:], in_=sc[:], func=AF.Silu), [B_sc], [B_sc])
                S.op("dve", lambda e: e.tensor_copy(out=screp[:], in_=sc[:].unsqueeze(2).to_broadcast([128, 16, 128])),
                     [B_sc], [B_screp])
                nrm_of = {1: 0, 2: 1, 4: 2, 5: 3}
                for k in range(6):
                    S.dma("sp", bad[:], b_ada[0:1, k * D:(k + 1) * D].to_broadcast([128, D]), [], [B_bad])
                    if k in nrm_of:
                        S.dma("sp", nrm[:], norms_d[nrm_of[k]:nrm_of[k] + 1, :].to_broadcast([128, D]), [], [B_nrm])
                    for n in range(4):
                        wt, wb = wad[(k * 4 + n) % 2]
                        c0 = k * D + n * 512
                        S.dma("pool", wt[:], w_ada[k * 4 + n], [], [wb])
                        pi = (k * 4 + n) % 2
                        S.mm(ps[pi][:], [(screp[:, kc, :], wt[:, kc, :]) for kc in range(16)], [wb, B_screp], [psb[pi]])
                        S.op("dve", lambda e: e.tensor_tensor(out=modt[:, n * 512:(n + 1) * 512], in0=ps[pi][:],
                                                              in1=bad[:, n * 512:(n + 1) * 512], op=ALU.add),
                             [psb[pi], B_bad], [B_modt])
                    if k in (1, 4):
                        S.op("dve", lambda e: e.scalar_tensor_tensor(out=modt[:], in0=modt[:], scalar=1.0, in1=nrm[:],
                                                                     op0=ALU.add, op1=ALU.mult), [B_modt, B_nrm], [B_modt])
                    if k in (2, 5):
                        S.op("dve", lambda e: e.tensor_tensor(out=modt[:], in0=modt[:], in1=nrm[:], op=ALU.mult),
                             [B_modt, B_nrm], [B_modt])
                    S.dma("sp", modrep[k], modt[:], [B_modt], [B_modrep[k]])
                S.barrier()

        def make_hT(xsrc, hT, B_hT, pstk):
            G1, B_G1 = sb("G1", [128, D], F32, pstk)
            SH1, B_SH1 = sb("SH1", [128, D], F32, pstk)
            xr = [sb(f"xin{i}", [128, D], F32, pstk) for i in range(2)]
            hb, B_hb = sb("hb", [128, D], BF16, pstk)
            junk, B_junk = sb("junk", [128, D], BF16, pstk)
            ss, B_ss = sb("ss", [128, 4], F32, pstk)
            S.dma("sp", G1[:], modrep[1], [B_modrep[1]], [B_G1])
            S.dma("sp", SH1[:], modrep[0], [B_modrep[0]], [B_SH1])
            for t in range(16):
                xin, B_x = xr[t % 2]
                S.dma("sp", xin[:], xsrc[t * 128:(t + 1) * 128, :], [], [B_x])
                rstd = rms_rstd(xin[:], B_x, junk[:], B_junk, ss, B_ss, D)
                S.op("dve", lambda e: e.scalar_tensor_tensor(out=xin[:], in0=xin[:], scalar=rstd, in1=G1[:],
                                                             op0=ALU.mult, op1=ALU.mult), [B_x, B_ss, B_G1], [B_x])
                S.op("dve", lambda e: e.tensor_tensor(out=hb[:], in0=xin[:], in1=SH1[:], op=ALU.add), [B_x, B_SH1], [B_hb])
                for half in range(2):
                    pi = 2 + half
                    pv = ps[pi][:].bitcast(BF16).rearrange("p (k t) -> p k t", t=128)
                    S.deps("pe", [B_hb, B_ident], [psb[pi]])
                    ins = None
                    for k in range(8):
                        kc = half * 8 + k
                        ins = nc.tensor.transpose(pv[:, k, :], hb[:, kc * 128:(kc + 1) * 128], ident_bf[:])
                    S.pe_ticket_after(ins, [B_hb, B_ident], [psb[pi]])
                    eng = "act" if half == 0 else "dve"
                    if eng == "act":
                        S.op("act", lambda e: e.copy(out=hT[:, half * 8:half * 8 + 8, t * 128:(t + 1) * 128], in_=pv),
                             [psb[pi]], [B_hT])
                    else:
                        S.op("dve", lambda e: e.tensor_copy(out=hT[:, half * 8:half * 8 + 8, t * 128:(t + 1) * 128], in_=pv),
                             [psb[pi]], [B_hT])

        KH3, B_KH3 = sb("KH3", [128, 4, 2048], BF16, hp)
        VH3, B_VH3 = sb("VH3", [128, 4, 16, 128], BF16, hp)
        KH2, B_KH2 = sb("KH2", [128, 4, 512], BF16, hp)
        VH2, B_VH2 = sb("VH2", [128, 4, 4, 128], BF16, hp)
        KH1, B_KH1 = sb("KH1", [128, 4, 128], BF16, hp)
        VH1, B_VH1 = sb("VH1", [128, 4, 1, 128], BF16, hp)
        SH, B_SH = sb("SHst", [128, 8, 128], F32, hp)
        KH = (KH1, KH2, KH3)
        B_KH = (B_KH1, B_KH2, B_KH3)
        VH = (VH1, VH2, VH3)
        B_VH = (B_VH1, B_VH2, B_VH3)


        def hgrn_front(hh, hT, t0, own, wts, tmp):
            wq, wf, wi, wg = wts
            (f1, B_f1), (f2, B_f2), (f3, B_f3), (f4, B_f4), (f5, B_f5) = tmp["f"]
            (kdT, B_kdT), (kbT, B_kbT), (qbT, B_qbT) = tmp["kq"]
            (kdk, B_kdk), (Vtok, B_Vtok), (eL, B_eL), (AT, B_AT) = tmp["m"]
            inproj_fm(wf, hT, B_hT, t0, 512, ps[0], psb[0])
            if own:
                inproj_fm(wq, hT, B_hT, t0, 512, ps[1], psb[1])
            pv3 = ps[3][:].rearrange("p (k t) -> p k t", t=128)
            for i in range(4):
                S.mm(pv3[:, i, :], [(hT[:, kc, t0 + i * 128:t0 + (i + 1) * 128], wi[0][:, kc, :]) for kc in range(16)],
                     [wi[1], B_hT], [psb[3]])
            if own:
                inproj_fm(wg, hT, B_hT, t0, 512, ps[7], psb[7])
            S.op("act", lambda e: e.activation(out=f1[:], in_=ps[0][:], func=AF.Sigmoid), [psb[0]], [B_f1])
            S.op("dve", lambda e: e.tensor_scalar(out=f2[:], in0=f1[:], scalar1=oml[:, hh:hh + 1], scalar2=lb[:, hh:hh + 1],
                                                  op0=ALU.mult, op1=ALU.add), [B_f1, B_oml, B_lb], [B_f2])
            yield
            S.op("act", lambda e: e.activation(out=f1[:], in_=f2[:], func=AF.Ln), [B_f2], [B_f1])
            S.op("dve", lambda e: e.tensor_tensor_scan(out=f3[:], data0=segm[:], data1=f1[:], initial=0.0,
                                                       op0=ALU.mult, op1=ALU.add), [B_f1, B_segm], [B_f3])
            b3 = f3[:].rearrange("p (c k) -> p c k", k=32)
            S.op("dve", lambda e: e.tensor_tensor(out=f1[:].rearrange("p (c k) -> p c k", k=32),
                                                  in0=b3[:, :, 31:32].to_broadcast([128, 16, 32]), in1=b3, op=ALU.subtract),
                 [B_f3], [B_f1])
            yield
            S.op("act", lambda e: e.activation(out=f4[:], in_=f1[:], func=AF.Exp), [B_f1], [B_f4])
            if own:
                S.op("act", lambda e: e.activation(out=f5[:], in_=f3[:], func=AF.Exp, scale=-1.0), [B_f3], [B_f5])
            S.op("act", lambda e: e.activation(out=eL[:].unsqueeze(2), in_=b3[:, :, 31:32], func=AF.Exp), [B_f3], [B_eL])
            S.op("dve", lambda e: e.tensor_scalar(out=f2[:], in0=f2[:], scalar1=-1.0, scalar2=1.0, op0=ALU.mult, op1=ALU.add),
                 [B_f2], [B_f2])
            S.op("dve", lambda e: e.tensor_tensor(out=kdT[:], in0=f2[:], in1=f4[:], op=ALU.mult), [B_f2, B_f4], [B_kdT])
            S.op("act", lambda e: e.copy(out=Vtok[:], in_=pv3), [psb[3]], [B_Vtok])
            Vm, B_Vm = tmp["vm"]
            for c in range(4):
                S.op("dve", lambda e: e.tensor_scalar(out=Vm[:, c, :, :], in0=Vtok[:], scalar1=rowm[:, c:c + 1], scalar2=None,
                                                      op0=ALU.mult), [B_Vtok, B_rowm], [B_Vm])
            yield
            if own:
                S.op("dve", lambda e: e.tensor_tensor(out=kbT[:], in0=f2[:], in1=f5[:], op=ALU.mult), [B_f2, B_f5], [B_kbT])
                S.op("act", lambda e: e.activation(out=f4[:], in_=f3[:], func=AF.Exp), [B_f3, B_kdT], [B_f4])
                S.op("dve", lambda e: e.tensor_tensor(out=qbT[:], in0=ps[1][:], in1=f4[:], op=ALU.mult), [psb[1], B_f4], [B_qbT])
                S.op("act", lambda e: e.activation(out=f5[:], in_=ps[7][:], func=AF.Silu), [psb[7], B_kbT], [B_f5])

        def hgrn_back(hh, t0, own, tmp, Sst, B_S, Sbf, curbox, ORT=None, B_ORT=None):
            cur = curbox[0]
            (f1, B_f1), (f2, B_f2), (f3, B_f3), (f4, B_f4), (f5, B_f5) = tmp["f"]
            (kdT, B_kdT), (kbT, B_kbT), (qbT, B_qbT) = tmp["kq"]
            (kdk, B_kdk), (Vtok, B_Vtok), (eL, B_eL), (AT, B_AT) = tmp["m"]
            Vm, B_Vm = tmp["vm"]
            pv = ps[2][:].bitcast(BF16)[:, 0:512].rearrange("p (k t) -> p k t", t=128)
            pA = ps[2][:, 256:384]
            S.deps("pe", [B_kdT, B_ident], [psb[2]])
            ins = None
            for i in range(4):
                ins = nc.tensor.transpose(pv[:, i, :], kdT[:, i * 128:(i + 1) * 128], ident_bf[:])
            S.pe_ticket_after(ins, [B_kdT, B_ident], [psb[2]])
            S.op("act", lambda e: e.copy(out=kdk[:], in_=pv), [psb[2]], [B_kdk])
            for i in range(4):
                if i > 0:
                    yield
                tsl = slice(i * 128, (i + 1) * 128)
                pU = ps[5 + (i % 2)]
                B_pU = psb[5 + (i % 2)]
                pUv = pU[:].rearrange("p (k t) -> p k t", t=128)
                for c in range(4):
                    S.mm(pUv[:, c, :], [(kdk[:, i, :], Vm[:, c, i, :])], [B_kdk, B_Vm], [B_pU])
                if own:
                    S.mm(pA, [(kbT[:, tsl], qbT[:, tsl])], [B_kbT, B_qbT], [psb[2]])
                    S.op("dve", lambda e: e.tensor_tensor(out=AT[:], in0=pA, in1=tri[:], op=ALU.mult),
                         [psb[2], B_tri], [B_AT])
                    S.mm(ps[4][:, tsl], [(Vtok[:, i, :], AT[:])], [B_Vtok, B_AT], [psb[4]], stop=False, skip_group_check=True)
                for c in range(4):
                    if own:
                        csl = slice(i * 128 + 32 * c, i * 128 + 32 * c + 32)
                        S.mm(ps[4][:, csl], [(Sbf[cur][0][:], qbT[:, csl])], [Sbf[cur][1], B_qbT], [psb[4]],
                             start=False, stop=True, skip_group_check=True)
                    S.op("dve", lambda e: e.scalar_tensor_tensor(out=Sst, in0=Sst, scalar=eL[:, i * 4 + c:i * 4 + c + 1],
                                                                 in1=pUv[:, c, :], op0=ALU.mult, op1=ALU.add),
                         [B_S, B_eL, B_pU], [B_S])
                    cur = (cur + 1) % len(Sbf)
                    if own:
                        S.op("dve", lambda e: e.tensor_copy(out=Sbf[cur][0][:], in_=Sst), [B_S], [Sbf[cur][1]])
            if own:
                (sq, B_sq), (o1, B_o1) = tmp["o"]
                S.op("act", lambda e: e.activation(out=sq[:], in_=ps[4][:], func=AF.Square), [psb[4]], [B_sq])
                S.mm(ps[0][:], [(ones_bf[:], sq[:])], [B_ones, B_sq], [psb[0]])
                S.op("dve", lambda e: e.tensor_scalar(out=f1[:], in0=ps[0][:], scalar1=1.0 / 128, scalar2=EPS,
                                                      op0=ALU.mult, op1=ALU.add), [psb[0]], [B_f1])
                S.op("act", lambda e: e.activation(out=f1[:], in_=f1[:], func=AF.Sqrt), [B_f1], [B_f1])
                S.op("dve", lambda e: e.reciprocal(out=f1[:], in_=f1[:]), [B_f1], [B_f1])
                S.op("dve", lambda e: e.tensor_tensor(out=o1[:], in0=ps[4][:], in1=f1[:], op=ALU.mult), [psb[4], B_f1], [B_o1])
                S.op("dve", lambda e: e.scalar_tensor_tensor(out=ORT[:, t0:t0 + 512], in0=o1[:], scalar=hn[:, hh:hh + 1],
                                                             in1=f5[:], op0=ALU.mult, op1=ALU.mult),
                     [B_o1, B_hn, B_f5], [B_ORT])
            curbox[0] = cur
            yield

        def hgrn_tmp(stk):
            return {
                "f": [sb(f"hf{i}", [128, 512], F32, stk) for i in range(5)],
                "kq": [sb(f"hk{i}", [128, 512], BF16, stk) for i in range(3)],
                "m": [sb("kdk", [128, 4, 128], BF16, stk), sb("Vtok", [128, 4, 128], BF16, stk),
                      sb("eL", [128, 16], F32, stk), sb("AT", [128, 128], BF16, stk)],
                "o": [sb("sq", [128, 512], BF16, stk), sb("o1", [128, 512], F32, stk)],
                "vm": sb("Vm", [128, 4, 4, 128], BF16, stk),
            }

        with ExitStack() as ph:
          if stage >= 0:
                with ExitStack() as ph0:
                    make_hT(x_halo, big, B_hT, ph0)
                    S.barrier()
                cpi = 0
                HSTOP = int(os.environ.get("HSTOP", "9"))
                for g in (range(3) if HSTOP >= 2 else []):
                    d = DIL[g]
                    span = 128 * d
                    for j in range(4):
                        hh = g * 4 + j
                        wk = load_wchunk(COL_KA + hh * 128)
                        wv = load_wchunk(COL_VA + hh * 128)
                        step = min(512, span)
                        for u0 in range(TOK - span, TOK, step):
                            pi = cpi % 2
                            cpi += 1
                            inproj_fm(wk, big, B_hT, u0, step, ps[pi], psb[pi])
                            o0 = u0 - (TOK - span)
                            S.op("act", lambda e: e.copy(out=KH[g][:, j, o0:o0 + step], in_=ps[pi][:, 0:step]), [psb[pi]], [B_KH[g]])
                        for r0 in range(0, d, 4):
                            nb = min(4, d - r0)
                            pi = 2 + (cpi % 2)
                            cpi += 1
                            pv = ps[pi][:].rearrange("p (k t) -> p k t", t=128)
                            for rr in range(nb):
                                r = r0 + rr
                                tsl = slice(TOK - span + r, TOK, d)
                                S.mm(pv[:, rr, :], [(big[:, kc, tsl], wv[0][:, kc, :]) for kc in range(16)], [wv[1], B_hT], [psb[pi]],
                                     ticket=True)
                            S.op("dve", lambda e: e.tensor_copy(out=VH[g][:, j, r0:r0 + nb, :], in_=pv[:, 0:nb, :]), [psb[pi]], [B_VH[g]])
                with ExitStack() as phh:
                    gfull, B_gf = sb("gfull", [128, TOK], F32, phh)
                    omf, B_omf = sb("omfull", [128, TOK], F32, phh)
                    Bc, B_Bc = sb("Bcum", [128, TOK], F32, phh)
                    kdTf, B_kdTf = sb("kdTf", [128, TOK], BF16, phh)
                    kdtok, B_kdtok = sb("kdtok", [128, 16, 128], BF16, phh)
                    Vth, B_Vth = sb("Vth", [128, 16, 128], BF16, phh)
                    fs = [sb(f"fsig{i}", [128, 512], F32, phh) for i in range(2)]
                    ones5, B_ones5 = sb("ones5", [128, 512], F32, phh)
                    S.op("dve", lambda e: e.memset(ones5[:], 1.0), [], [B_ones5])
                    S.op("dve", lambda e: e.memset(SH[:], 0.0), [], [B_SH])
                    for hh in (range(8) if HSTOP >= 3 else []):
                        wf = load_wchunk(COL_FR + hh * 128)
                        wi = load_wchunk(COL_IR + hh * 128)
                        for seg in range(4):
                            t0 = seg * 512
                            tsl = slice(t0, t0 + 512)
                            pf_, B_pf = ps[seg % 2], psb[seg % 2]
                            f1, B_f1 = fs[seg % 2]
                            inproj_fm(wf, big, B_hT, t0, 512, pf_, B_pf)
                            S.op("act", lambda e: e.activation(out=f1[:], in_=pf_[:], func=AF.Sigmoid), [B_pf], [B_f1])
                            S.op("dve", lambda e: e.tensor_scalar(out=omf[:, tsl], in0=f1[:], scalar1=oml[:, hh:hh + 1],
                                                                  scalar2=lb[:, hh:hh + 1], op0=ALU.mult, op1=ALU.add),
                                 [B_f1, B_oml, B_lb], [B_omf])
                            S.op("act", lambda e: e.activation(out=gfull[:, tsl], in_=omf[:, tsl], func=AF.Ln), [B_omf], [B_gf])
                            S.op("dve", lambda e: e.tensor_scalar(out=omf[:, tsl], in0=omf[:, tsl], scalar1=-1.0, scalar2=1.0,
                                                                  op0=ALU.mult, op1=ALU.add), [B_omf, B_gf], [B_omf])
                            init = 0.0 if seg == 0 else Bc[:, t0 - 1:t0]
                            S.op("dve", lambda e: e.tensor_tensor_scan(out=Bc[:, tsl], data0=ones5[:], data1=gfull[:, tsl], initial=init,
                                                                       op0=ALU.mult, op1=ALU.add), [B_gf, B_ones5, B_Bc], [B_Bc])
                            pvb = ps[2 + seg % 2]
                            B_pvb = psb[2 + seg % 2]
                            pv3 = pvb[:].rearrange("p (k t) -> p k t", t=128)
                            for i in range(4):
                                S.mm(pv3[:, i, :], [(big[:, kc, t0 + i * 128:t0 + (i + 1) * 128], wi[0][:, kc, :]) for kc in range(16)],
                                     [wi[1], B_hT], [B_pvb])
                            if seg % 2 == 0:
                                S.op("act", lambda e: e.copy(out=Vth[:, seg * 4:seg * 4 + 4, :], in_=pv3), [B_pvb], [B_Vth])
                            else:
                                S.op("dve", lambda e: e.tensor_copy(out=Vth[:, seg * 4:seg * 4 + 4, :], in_=pv3), [B_pvb], [B_Vth])
                        S.op("dve", lambda e: e.tensor_tensor(out=gfull[:], in0=Bc[:, TOK - 1:TOK].to_broadcast([128, TOK]), in1=Bc[:],
                                                              op=ALU.subtract), [B_Bc, B_gf], [B_gf])
                        S.op("act", lambda e: e.activation(out=gfull[:], in_=gfull[:], func=AF.Exp), [B_gf], [B_gf])
                        S.op("dve", lambda e: e.tensor_tensor(out=kdTf[:], in0=omf[:], in1=gfull[:], op=ALU.mult), [B_omf, B_gf], [B_kdTf])
                        for half in range(2):
                            pi = 4 + half
                            pv = ps[pi][:].bitcast(BF16).rearrange("p (k t) -> p k t", t=128)
                            S.deps("pe", [B_kdTf, B_ident], [psb[pi]])
                            ins = None
                            for k in range(8):
                                i = half * 8 + k
                                ins = nc.tensor.transpose(pv[:, k, :], kdTf[:, i * 128:(i + 1) * 128], ident_bf[:])
                            S.pe_ticket_after(ins, [B_kdTf, B_ident], [psb[pi]])
                            if half == 0:
                                S.op("act", lambda e: e.copy(out=kdtok[:, 0:8, :], in_=pv), [psb[pi]], [B_kdtok])
                            else:
                                S.op("dve", lambda e: e.tensor_copy(out=kdtok[:, 8:16, :], in_=pv), [psb[pi]], [B_kdtok])
                        S.mm(ps[6][:, 0:128], [(kdtok[:, i, :], Vth[:, i, :]) for i in range(16)], [B_kdtok, B_Vth], [psb[6]])
                        S.op("dve", lambda e: e.tensor_copy(out=SH[:, hh, :], in_=ps[6][:, 0:128]), [psb[6]], [B_SH])
                    S.op("dve", lambda e: e.tensor_scalar(out=SH[:], in0=SH[:], scalar1=flag[:, 0:1], scalar2=None, op0=ALU.mult),
                         [B_SH, B_flag], [B_SH])
                S.barrier()

        with ExitStack() as pb:
            if stage >= 1:
                make_hT(x_own, big, B_hT, pb)
            S.barrier()
        if debug and stage >= 1:
            with ExitStack() as pd:
                dt_, B_dt = sb("dbgt", [128, 16, 512], F32, pd)
                for s4 in range(4):
                    S.op("dve", lambda e: e.tensor_copy(out=dt_[:], in_=big[:, :, s4 * 512:(s4 + 1) * 512]), [B_hT], [B_dt])
                    S.dma("sp", dbg["d_h"][:, :, s4 * 512:(s4 + 1) * 512], dt_[:], [B_dt], [])
                S.barrier()

        if stage >= 2:
            with ExitStack() as pdd:
                QT, B_QT = sb("QT", [128, TOK], BF16, pdd)
                KT, B_KT = sb("KT", [128, TOK], BF16, pdd)
                VB, B_VB = sb("VB", [128, 16, 128], BF16, pdd)
                ND, B_NUM = sb("ND", [128, 2, TOK], F32, pdd)
                B_DEN = B_NUM
                NUM = ND[:, 0, :]
                DEN = ND[:, 1, :]
                OA, B_OA = sb("OA", [128, TOK], BF16, pdd)
                ebt, B_ebt = sb("ebt", [128, 256], F32, pdd)
                ebf, B_ebf = sb("ebf", [128, 256], F32, pdd)
                esb = [sb(f"esb{i}", [128, 256], F32, pdd) for i in range(2)]
                Pt = [sb(f"Pt{i}", [128, 256], BF16, pdd) for i in range(2)]
                cpi = 0
                for j in range(4):
                    for g in range(3):
                        d = DIL[g]
                        span = 128 * d
                        hh = g * 4 + j
                        S.dma("sp", ebt[:], alibi_d[hh], [], [B_ebt])
                        S.op("dve", lambda e: e.tensor_copy(out=ebf[:, 128:256], in_=ebt[:, 128:256]), [B_ebt], [B_ebf])
                        S.op("dve", lambda e: e.tensor_scalar(out=ebf[:, 0:128], in0=ebt[:, 0:128], scalar1=flag[:, 0:1],
                                                              scalar2=None, op0=ALU.mult), [B_ebt, B_flag], [B_ebf])
                        wq = load_wchunk(COL_QA + hh * 128)
                        wk = load_wchunk(COL_KA + hh * 128)
                        wv = load_wchunk(COL_VA + hh * 128)
                        for s4 in range(4):
                            pi = cpi % 2
                            cpi += 1
                            inproj_fm(wq, big, B_hT, s4 * 512, 512, ps[pi], psb[pi])
                            S.op("act", lambda e: e.copy(out=QT[:, s4 * 512:(s4 + 1) * 512], in_=ps[pi][:]), [psb[pi]], [B_QT])
                            pi = cpi % 2
                            cpi += 1
                            inproj_fm(wk, big, B_hT, s4 * 512, 512, ps[pi], psb[pi])
                            S.op("dve", lambda e: e.tensor_copy(out=KT[:, s4 * 512:(s4 + 1) * 512], in_=ps[pi][:]), [psb[pi]], [B_KT])
                        for b0 in range(0, 16, 4):
                            pi = 2 + (cpi % 2)
                            cpi += 1
                            pv = ps[pi][:].rearrange("p (k t) -> p k t", t=128)
                            for bb in range(4):
                                blk = b0 + bb
                                nsp, r = blk // d, blk % d
                                tsl = slice(nsp * span + r, (nsp + 1) * span, d)
                                S.mm(pv[:, bb, :], [(big[:, kc, tsl], wv[0][:, kc, :]) for kc in range(16)], [wv[1], B_hT],
                                     [psb[pi]], ticket=True)
                            S.op("act", lambda e: e.copy(out=VB[:, b0:b0 + 4, :], in_=pv), [psb[pi]], [B_VB])
                        DSTOP = int(os.environ.get("DSTOP", "9"))
                        for blk in (range(16) if DSTOP >= 2 else []):
                            nsp, r = blk // d, blk % d
                            qsl = slice(nsp * span + r, (nsp + 1) * span, d)
                            if nsp == 0:
                                kprev = KH[g][:, j, r:span:d]
                                vprev = VH[g][:, j, r, :]
                                rdk = [B_KH[g]]
                                rdv = [B_VH[g]]
                                tab, B_tab = ebf, B_ebf
                            else:
                                kprev = KT[:, (nsp - 1) * span + r:nsp * span:d]
                                vprev = VB[:, (nsp - 1) * d + r, :]
                                rdk = []
                                rdv = []
                                tab, B_tab = ebt, B_ebt
                            bi = blk % 2
                            pS, B_pS = ps[4 + bi], psb[4 + bi]
                            pO, B_pO = ps[6 + bi], psb[6 + bi]
                            S.mm(pS[:, 0:128], [(kprev, QT[:, qsl])], [B_QT, B_KT] + rdk, [B_pS], ticket=True)
                            S.mm(pS[:, 128:256], [(KT[:, qsl], QT[:, qsl])], [B_QT, B_KT] + rdk, [B_pS])
                            et, B_et = esb[bi]
                            pt, B_pt = Pt[bi]
                            S.op("act", lambda e: e.activation(out=et[:], in_=pS[:, 0:256], func=AF.Exp, scale=128 ** -0.5),
                                 [B_pS], [B_et])
                            S.op("dve", lambda e: e.tensor_tensor(out=pt[:], in0=et[:], in1=tab[:], op=ALU.mult),
                                 [B_et, B_tab], [B_pt])
                            if DSTOP < 3:
                                continue
                            S.mm(pO[:, 0:128], [(vprev, pt[:, 0:128]), (VB[:, blk, :], pt[:, 128:256])], [B_pt, B_VB] + rdv,
                                 [B_pO], ticket=True)
                            S.mm(pO[:, 128:256], [(ones_bf[:], pt[:, 0:128]), (ones_bf[:], pt[:, 128:256])], [B_pt, B_ones], [B_pO])
                            if DSTOP < 4:
                                continue
                            pOv = pO[:, 0:256].rearrange("p (a q) -> p a q", a=2)
                            if g == 0:
                                S.op("dve", lambda e: e.tensor_copy(out=ND[:, :, qsl], in_=pOv), [B_pO], [B_NUM])
                            else:
                                S.op("dve", lambda e: e.tensor_tensor(out=ND[:, :, qsl], in0=ND[:, :, qsl], in1=pOv, op=ALU.add),
                                     [B_pO, B_NUM], [B_NUM])
                    S.op("dve", lambda e: e.reciprocal(out=DEN, in_=DEN), [B_DEN], [B_DEN])
                    S.op("dve", lambda e: e.tensor_tensor(out=OA[:], in0=NUM, in1=DEN, op=ALU.mult), [B_NUM], [B_OA])
                    S.dma("sp", oa_scr[:, j, :], OA[:], [B_OA], [B_oascr[j]])
                S.barrier()

        if stage >= 3:
            with ExitStack() as pe_:
                tmps = [hgrn_tmp(pe_), hgrn_tmp(pe_)]
                Sbf = [sb(f"Sbf{i}", [128, 128], BF16, pe_) for i in range(8)]
                orts = [sb(f"ORT{i}", [128, TOK], BF16, pe_) for i in range(2)]
                units = [(hh, seg) for hh in range(8) for seg in range(4)]
                wts_h = {}
                curbox = [0]

                def drain(g):
                    for _ in g:
                        pass

                for ui in range(len(units) + 1):
                    fr = None
                    bk = None
                    if ui < len(units):
                        hh, seg = units[ui]
                        if seg == 0:
                            wts_h[hh] = (load_wchunk(COL_QR + hh * 128), load_wchunk(COL_FR + hh * 128),
                                         load_wchunk(COL_IR + hh * 128), load_wchunk(COL_GR + hh * 128))
                        fr = hgrn_front(hh, big, seg * 512, True, wts_h[hh], tmps[ui % 2])
                    if ui > 0:
                        ph_, ps_ = units[ui - 1]
                        ORT, B_ORT = orts[ph_ % 2]
                        if ps_ == 0:
                            S.op("act", lambda e: e.copy(out=Sbf[0][0][:], in_=SH[:, ph_, :]), [B_SH], [Sbf[0][1]])
                            curbox[0] = 0
                        bk = hgrn_back(ph_, ps_ * 512, True, tmps[(ui - 1) % 2], SH[:, ph_, :], B_SH, Sbf, curbox, ORT, B_ORT)
                    if fr is not None:
                        drain(fr)
                    if bk is not None:
                        drain(bk)
                    if ui > 0 and ps_ == 3:
                        S.dma("sp", oa_scr[:, 4 + ph_, :], ORT[:], [B_ORT], [B_oascr[4 + ph_]])
                S.barrier()
        if debug and stage >= 2:
            with ExitStack() as pd:
                db_, B_db = sb("dbgb", [128, 12, 512], BF16, pd)
                dt_, B_dt = sb("dbgt", [128, 12, 512], F32, pd)
                for s4 in range(4):
                    S.dma("sp", db_[:], oa_scr[:, :, s4 * 512:(s4 + 1) * 512], B_oascr, [B_db])
                    S.op("dve", lambda e: e.tensor_copy(out=dt_[:], in_=db_[:]), [B_db], [B_dt])
                    S.dma("sp", dbg["d_oa"][:, :, s4 * 512:(s4 + 1) * 512], dt_[:], [B_dt], [])
                S.barrier()

        hp.close()
        if stage >= 4:
            with ExitStack() as pf:
                OAall, B_OAall = sb("OAall", [128, 12, TOK], BF16, pf)
                wbr = [sb(f"wbr{i}", [128, 12, 128], BF16, pf) for i in range(2)]
                sg = [sb(f"sg{i}", [128, 512], F32, pf) for i in range(2)]
                m1, B_m1 = sb("m1", [128, 512], F32, pf)
                m2, B_m2 = sb("m2", [128, 512], F32, pf)
                mtc = [sb(f"mtc{i}", [128, TOK], BF16, pf) for i in range(2)]
                S.dma("sp", OAall[:], oa_scr, B_oascr, [B_OAall])
                for c in range(16):
                    wga = load_wchunk(COL_GA + c * 128)
                    wgb = load_wchunk(COL_GB + c * 128)
                    wb_t, B_wb = wbr[c % 2]
                    S.dma("pool", wb_t[:, 0:4, :], w_ba[c], [], [B_wb])
                    S.dma("pool", wb_t[:, 4:12, :], w_bh[c], [], [B_wb])
                    MTc, B_MTc = mtc[c % 2]
                    for s4 in range(4):
                        t0 = s4 * 512
                        inproj_fm(wga, big, B_hT, t0, 512, ps[0], psb[0])
                        S.op("act", lambda e: e.activation(out=sg[0][0][:], in_=ps[0][:], func=AF.Sigmoid), [psb[0]], [sg[0][1]])
                        S.mm(ps[1][:], [(wb_t[:, jj, :], OAall[:, jj, t0:t0 + 512]) for jj in range(4)], [B_wb, B_OAall], [psb[1]])
                        S.op("dve", lambda e: e.tensor_tensor(out=m1[:], in0=sg[0][0][:], in1=ps[1][:], op=ALU.mult),
                             [sg[0][1], psb[1]], [B_m1])
                        inproj_fm(wgb, big, B_hT, t0, 512, ps[2], psb[2])
                        S.op("act", lambda e: e.activation(out=sg[1][0][:], in_=ps[2][:], func=AF.Sigmoid), [psb[2]], [sg[1][1]])
                        S.mm(ps[3][:], [(wb_t[:, jj, :], OAall[:, jj, t0:t0 + 512]) for jj in range(4, 12)], [B_wb, B_OAall], [psb[3]])
                        S.op("dve", lambda e: e.tensor_tensor(out=m2[:], in0=sg[1][0][:], in1=ps[3][:], op=ALU.mult),
                             [sg[1][1], psb[3]], [B_m2])
                        S.op("dve", lambda e: e.tensor_tensor(out=MTc[:, t0:t0 + 512], in0=m1[:], in1=m2[:], op=ALU.add),
                             [B_m1, B_m2], [B_MTc])
                    S.dma("sp", mt_scr.rearrange("t p c k -> p c t k")[:, c], MTc[:].rearrange("p (t k) -> p t k", k=128),
                          [B_MTc], [B_mtscr])
                S.barrier()
        mid.close()
        if stage >= 4:
            with ExitStack() as pf:
                Wo, B_Wo = sb("Wo", [128, 16, D], BF16, pf)
                GMt, B_GMt = sb("GMt", [128, D], F32, pf)
                G2t, B_G2t = sb("G2t", [128, D], F32, pf)
                SH2t, B_SH2t = sb("SH2t", [128, D], F32, pf)
                mtt = [sb(f"mtt{i}", [128, 16, 128], BF16, pf) for i in range(2)]
                xr = [sb(f"xf{i}", [128, D], F32, pf) for i in range(2)]
                tmpf_r = [sb(f"tmpf{i}", [128, D], F32, pf) for i in range(2)]
                h2b_r = [sb(f"h2b{i}", [128, D], BF16, pf) for i in range(2)]
                h2t_r = [sb(f"h2tt{i}", [128, 16, 128], BF16, pf) for i in range(2)]
                junk_r = [sb(f"junkf{i}", [128, D], BF16, pf) for i in range(2)]
                ss_r = [sb(f"ssf{i}", [128, 4], F32, pf) for i in range(2)]
                m1_r = [sb(f"m1s{i}", [128, 8], F32, pf) for i in range(2)]
                yb_r = [sb(f"ybuf{i}", [128, D], F32, pf) for i in range(2)]
                yb2_b = [Buf("yb2a"), Buf("yb2b")]
                wrt, B_wrt = sb("wrt", [128, 16, NE], BF16, pf)
                rt = {k: sb("rt_" + k, shp, F32, pf) for k, shp in
                      (("sc", [128, 64]), ("ch", [128, 64]), ("m1", [128, 8]), ("eq", [128, 64]), ("ch2", [128, 64]),
                       ("m2", [128, 8]), ("gs", [128, 8]), ("t8", [128, 8]), ("gm", [128, 8]), ("chm", [128, 64]),
                       ("t8e", [128, 8]), ("sel", [128, 64]), ("gsel", [128, 64]), ("den", [128, 2]))}
                S.dma("pool", wrt[:], w_r, [], [B_wrt])
                for pc in range(8):
                    S.dma("pool", Wo[:, :, pc * 256:(pc + 1) * 256], w_out[pc], [], [B_Wo])
                S.dma("sp", GMt[:], modrep[2], [B_modrep[2]], [B_GMt])
                S.dma("sp", G2t[:], modrep[4], [B_modrep[4]], [B_G2t])
                S.dma("sp", SH2t[:], modrep[3], [B_modrep[3]], [B_SH2t])
                def f2_outproj(tile_i):
                    MTt, B_MTt = mtt[tile_i % 2]
                    S.dma("sp", MTt[:], mt_scr[tile_i], [B_mtscr], [B_MTt])
                    for n in range(4):
                        S.mm(ps[4 + n][:], [(MTt[:, kc, :], Wo[:, kc, n * 512:(n + 1) * 512]) for kc in range(16)],
                             [B_MTt, B_Wo], [psb[4 + n]])

                f2_outproj(0)
                for tile_i in range(16):
                    r0 = tile_i * 128
                    tmpf, B_tmpf = tmpf_r[tile_i % 2]
                    h2b, B_h2b = h2b_r[tile_i % 2]
                    h2t, B_h2tt = h2t_r[tile_i % 2]
                    junk, B_junk = junk_r[tile_i % 2]
                    ss, B_ss = ss_r[tile_i % 2]
                    m1, B_m1 = m1_r[tile_i % 2]
                    xin, B_x = xr[tile_i % 2]
                    S.dma("sp", xin[:], x_own[r0:r0 + 128, :], [], [B_x])
                    yb, B_yb = yb_r[tile_i % 2]
                    B_yb2 = yb2_b[tile_i % 2]
                    for n in range(4):
                        nsl = slice(n * 512, (n + 1) * 512)
                        if n % 2 == 0:
                            S.op("act", lambda e: e.copy(out=yb[:, nsl], in_=ps[4 + n][:]), [psb[4 + n]], [B_yb])
                        else:
                            S.op("dve", lambda e: e.tensor_copy(out=yb[:, nsl], in_=ps[4 + n][:]), [psb[4 + n]], [B_yb2])
                    S.op("dve", lambda e: e.memset(ss[:, 0:1], 0.0), [], [B_ss])
                    S.op("act", lambda e: e.activation(out=junk[:], in_=yb[:], func=AF.Square, accum_out=ss[:, 0:1]),
                         [B_yb, B_yb2], [B_junk, B_ss])
                    S.op("dve", lambda e: e.tensor_scalar(out=ss[:, 1:2], in0=ss[:, 0:1], scalar1=1.0 / D, scalar2=EPS,
                                                          op0=ALU.mult, op1=ALU.add), [B_ss], [B_ss])
                    S.op("act", lambda e: e.activation(out=ss[:, 2:3], in_=ss[:, 1:2], func=AF.Sqrt), [B_ss], [B_ss])
                    S.op("dve", lambda e: e.reciprocal(out=ss[:, 3:4], in_=ss[:, 2:3]), [B_ss], [B_ss])
                    S.op("dve", lambda e: e.scalar_tensor_tensor(out=tmpf[:], in0=yb[:], scalar=ss[:, 3:4], in1=GMt[:],
                                                                 op0=ALU.mult, op1=ALU.mult), [B_yb, B_yb2, B_ss, B_GMt], [B_tmpf])
                    S.op("dve", lambda e: e.tensor_tensor(out=xin[:], in0=xin[:], in1=tmpf[:], op=ALU.add), [B_x, B_tmpf], [B_x])
                    S.dma("pool", out_d[r0:r0 + 128, :], xin[:], [B_x], [B_out[tile_i]])
                    if debug:
                        S.dma("sp", dbg["d_x1"][r0:r0 + 128, :], xin[:], [B_x], [])
                    if tile_i < 15:
                        f2_outproj(tile_i + 1)
                    rstd = rms_rstd(xin[:], B_x, junk[:], B_junk, ss, B_ss, D)
                    S.op("dve", lambda e: e.scalar_tensor_tensor(out=tmpf[:], in0=xin[:], scalar=rstd, in1=G2t[:],
                                                                 op0=ALU.mult, op1=ALU.mult), [B_x, B_ss, B_G2t], [B_tmpf])
                    S.op("dve", lambda e: e.tensor_tensor(out=h2b[:], in0=tmpf[:], in1=SH2t[:], op=ALU.add), [B_tmpf, B_SH2t], [B_h2b])
                    for half in range(2):
                        pi = 2 + half
                        pv = ps[pi][:].bitcast(BF16).rearrange("p (k t) -> p k t", t=128)
                        S.deps("pe", [B_h2b, B_ident], [psb[pi]])
                        ins = None
                        for k in range(8):
                            kc = half * 8 + k
                            ins = nc.tensor.transpose(pv[:, k, :], h2b[:, kc * 128:(kc + 1) * 128], ident_bf[:])
                        S.pe_ticket_after(ins, [B_h2b, B_ident], [psb[pi]])
                        S.op("act", lambda e: e.copy(out=h2t[:, half * 8:half * 8 + 8, :], in_=pv), [psb[pi]], [B_h2tt])
                    S.dma("pool", h2t_scr[:, :, r0:r0 + 128], h2t[:], [B_h2tt], [B_h2t[tile_i]])
                    S.mm(ps[0][:, 0:NE], [(h2t[:, kc, :], wrt[:, kc, :]) for kc in range(16)], [B_h2tt, B_wrt], [psb[0]])
                    R = lambda k: rt[k][0]
                    RB = lambda k: rt[k][1]
                    v3 = lambda a: a.rearrange("p (g k) -> p g k", k=8)
                    S.op("act", lambda e: e.activation(out=R("sc")[:], in_=ps[0][:, 0:NE], func=AF.Sigmoid), [psb[0]], [RB("sc")])
                    S.op("dve", lambda e: e.tensor_tensor(out=R("ch")[:], in0=R("sc")[:], in1=rbias[:], op=ALU.add),
                         [RB("sc"), B_rbias], [RB("ch")])
                    S.op("dve", lambda e: e.tensor_reduce(out=R("m1")[:], in_=v3(R("ch")[:]), axis=AX.X, op=ALU.max),
                         [RB("ch")], [RB("m1")])
                    S.op("dve", lambda e: e.tensor_tensor(out=v3(R("eq")[:]), in0=v3(R("ch")[:]),
                                                          in1=R("m1")[:].unsqueeze(2).to_broadcast([128, 8, 8]), op=ALU.is_equal),
                         [RB("ch"), RB("m1")], [RB("eq")])
                    S.op("dve", lambda e: e.scalar_tensor_tensor(out=R("ch2")[:], in0=R("eq")[:], scalar=-1e9, in1=R("ch")[:],
                                                                 op0=ALU.mult, op1=ALU.add), [RB("eq"), RB("ch")], [RB("ch2")])
                    S.op("dve", lambda e: e.tensor_reduce(out=R("m2")[:], in_=v3(R("ch2")[:]), axis=AX.X, op=ALU.max),
                         [RB("ch2")], [RB("m2")])
                    S.op("dve", lambda e: e.tensor_tensor(out=R("gs")[:], in0=R("m1")[:], in1=R("m2")[:], op=ALU.add),
                         [RB("m1"), RB("m2")], [RB("gs")])
                    S.op("dve", lambda e: e.max(out=R("t8")[:], in_=R("gs")[:]), [RB("gs")], [RB("t8")])
                    S.op("dve", lambda e: e.tensor_scalar(out=R("gm")[:], in0=R("gs")[:], scalar1=R("t8")[:, 3:4], scalar2=None,
                                                          op0=ALU.is_ge), [RB("gs"), RB("t8")], [RB("gm")])
                    S.op("dve", lambda e: e.tensor_scalar(out=R("gm")[:], in0=R("gm")[:], scalar1=1e9, scalar2=-1e9,
                                                          op0=ALU.mult, op1=ALU.add), [RB("gm")], [RB("gm")])
                    S.op("dve", lambda e: e.tensor_tensor(out=v3(R("chm")[:]), in0=v3(R("ch")[:]),
                                                          in1=R("gm")[:].unsqueeze(2).to_broadcast([128, 8, 8]), op=ALU.add),
                         [RB("ch"), RB("gm")], [RB("chm")])
                    S.op("dve", lambda e: e.max(out=R("t8e")[:], in_=R("chm")[:]), [RB("chm")], [RB("t8e")])
                    S.op("dve", lambda e: e.tensor_scalar(out=R("sel")[:], in0=R("chm")[:], scalar1=R("t8e")[:, 7:8], scalar2=None,
                                                          op0=ALU.is_ge), [RB("chm"), RB("t8e")], [RB("sel")])
                    S.op("dve", lambda e: e.tensor_tensor(out=R("gsel")[:], in0=R("sel")[:], in1=R("sc")[:], op=ALU.mult),
                         [RB("sel"), RB("sc")], [RB("gsel")])
                    S.op("dve", lambda e: e.tensor_reduce(out=R("den")[:, 0:1], in_=R("gsel")[:], axis=AX.X, op=ALU.add),
                         [RB("gsel")], [RB("den")])
                    S.op("dve", lambda e: e.reciprocal(out=R("den")[:, 1:2], in_=R("den")[:, 0:1]), [RB("den")], [RB("den")])
                    S.op("dve", lambda e: e.tensor_scalar(out=Gt[:, tile_i, 0:NE], in0=R("gsel")[:], scalar1=R("den")[:, 1:2],
                                                          scalar2=2.5, op0=ALU.mult, op1=ALU.mult), [RB("gsel"), RB("den")], [B_Gt])
                if debug:
                    S.dma("sp", dbg["d_g"], Gt[:], [B_Gt], [])
                S.barrier()
        mid.close()
        if stage >= 5:
            with ExitStack() as pg:
                h2p, B_h2p = sb("h2p", [128, 16, 1024], BF16, pg)
                yacc, B_yacc = sb("yacc", [128, 8, D], F32, pg)
                ss, B_ss = sb("ssg", [128, 4], F32, pg)
                for p in range(2):
                    S.dma("sp", h2p[:], h2t_scr[:, :, p * 1024:(p + 1) * 1024], B_h2t, [B_h2p])
                    with ExitStack() as pw:
                        xgu = [(sb(f"xg{i}", [128, 16, 256], BF16, pw), sb(f"xu{i}", [128, 16, 256], BF16, pw)) for i in range(3)]
                        xdr = [sb(f"xd{i}", [128, 4, D], BF16, pw) for i in range(2)]
                        hid = [[sb(f"hid{a}{t}", [128, 4, 512], BF16, pw) for t in range(2)] for a in range(2)]
                        sgl = [sb(f"sgl{i}", [128, 512], F32, pw) for i in range(2)]
                        di = [0]

                        def emit_down(ex, regions):
                            xd, B_xd = xdr[ex % 2]
                            for (tg, i, n) in regions:
                                hd_, B_hd = hid[ex % 2][tg]
                                lt = tg * 4 + i
                                pi = 4 + (di[0] % 4)
                                di[0] += 1
                                nsl = slice(n * 512, (n + 1) * 512)
                                S.mm(ps[pi][:], [(hd_[:, kc, i * 128:(i + 1) * 128], xd[:, kc, nsl]) for kc in range(4)],
                                     [B_hd, B_xd], [psb[pi]])
                                if ex == 0:
                                    S.op("dve", lambda e: e.tensor_scalar(out=yacc[:, lt, nsl], in0=ps[pi][:],
                                                                          scalar1=Gt[:, p * 8 + lt, ex:ex + 1], scalar2=None,
                                                                          op0=ALU.mult), [psb[pi], B_Gt], [B_yacc])
                                else:
                                    S.op("dve", lambda e: e.scalar_tensor_tensor(out=yacc[:, lt, nsl], in0=ps[pi][:],
                                                                                 scalar=Gt[:, p * 8 + lt, ex:ex + 1], in1=yacc[:, lt, nsl],
                                                                                 op0=ALU.mult, op1=ALU.add),
                                         [psb[pi], B_Gt, B_yacc], [B_yacc])

                        allreg = [(tg, i, n) for tg in range(2) for i in range(4) for n in range(4)]
                        for ex in range(NE + 1):
                            gsrc = w_ge[ex] if ex < NE else w_gs
                            usrc = w_ue[ex] if ex < NE else w_us
                            dsrc = w_de[ex] if ex < NE else w_ds
                            slots = []
                            for hf in range(2):
                                (xg, B_xg), (xu, B_xu) = xgu[(2 * ex + hf) % 3]
                                hsl = slice(hf * 256, (hf + 1) * 256)
                                S.dma("pool", xg[:], gsrc[hf], [], [B_xg])
                                S.dma("pool", xu[:], usrc[hf], [], [B_xu])
                                slots.append(((xg, B_xg), (xu, B_xu)))
                            xd, B_xd = xdr[ex % 2]
                            S.dma("pool", xd[:], dsrc, [], [B_xd])
                            u = 0
                            for hf in range(2):
                                (xg, B_xg), (xu, B_xu) = slots[hf]
                                for tg in range(2):
                                    tsl = slice(tg * 512, (tg + 1) * 512)
                                    hd_, B_hd = hid[ex % 2][tg]
                                    for ch in range(2):
                                        csl = slice(ch * 128, (ch + 1) * 128)
                                        S.mm(ps[ch][:], [(xg[:, kc, csl], h2p[:, kc, tsl]) for kc in range(16)], [B_xg, B_h2p], [psb[ch]])
                                        S.mm(ps[2 + ch][:], [(xu[:, kc, csl], h2p[:, kc, tsl]) for kc in range(16)], [B_xu, B_h2p], [psb[2 + ch]])
                                    for ch in range(2):
                                        sg_, B_sg = sgl[ch]
                                        S.op("act", lambda e: e.activation(out=sg_[:], in_=ps[ch][:], func=AF.Silu), [psb[ch]], [B_sg])
                                        S.op("dve", lambda e: e.tensor_tensor(out=hd_[:, hf * 2 + ch, :], in0=sg_[:], in1=ps[2 + ch][:], op=ALU.mult),
                                             [B_sg, psb[2 + ch]], [B_hd])
                                    if ex > 0:
                                        emit_down(ex - 1, allreg[u * 8:(u + 1) * 8])
                                    u += 1
                        emit_down(NE, allreg)
                        S.barrier()
                    with ExitStack() as pz:
                        xin_r = [sb(f"xfin{i}", [128, D], F32, pz) for i in range(3)]
                        mrep, B_mrep = sb("mrepg", [128, D], F32, pz)
                        junk_r = [sb(f"junkg{i}", [128, D], BF16, pz) for i in range(2)]
                        ss_r = [sb(f"ssg{i}", [128, 4], F32, pz) for i in range(2)]
                        S.dma("sp", mrep[:], modrep[5], [B_modrep[5]], [B_mrep])
                        B_yt = [Buf(f"yacc_t{p}_{i}") for i in range(8)]
                        for lt in range(8):
                            r0 = (p * 8 + lt) * 128
                            B_yacc_l = B_yt[lt]
                            xin, B_x = xin_r[lt % 3]
                            junk, B_junk = junk_r[lt % 2]
                            ss, B_ss = ss_r[lt % 2]
                            S.dma("pool", xin[:], out_d[r0:r0 + 128, :], [B_out[p * 8 + lt]], [B_x])
                            rstd = rms_rstd(yacc[:, lt, :], B_yacc_l, junk[:], B_junk, ss, B_ss, D)
                            S.op("dve", lambda e: e.scalar_tensor_tensor(out=yacc[:, lt, :], in0=yacc[:, lt, :], scalar=rstd, in1=mrep[:],
                                                                         op0=ALU.mult, op1=ALU.add if False else ALU.mult),
                                 [B_yacc_l, B_ss, B_mrep], [B_yacc_l])
                            S.op("dve", lambda e: e.tensor_tensor(out=xin[:], in0=xin[:], in1=yacc[:, lt, :], op=ALU.add),
                                 [B_x, B_yacc_l], [B_x])
                            S.dma("sp", out_d[r0:r0 + 128, :], xin[:], [B_x], [B_out[p * 8 + lt]])
                        S.barrier()
        S.barrier()
    return nc


def _consts():
    ident = np.eye(128, dtype=np.float32)
    s = np.arange(128)[:, None]
    t = np.arange(128)[None, :]
    tri = ((s // 32 == t // 32) & (s <= t)).astype(np.float32)
    seg = np.ones((128, 512), np.float32)
    seg[:, ::32] = 0.0
    slopes = np.exp2(-8.0 * np.arange(1, 13, dtype=np.float64) / 12)
    alibi = np.zeros((12, 128, 256), np.float32)
    k = np.arange(128)[:, None].astype(np.float64)
    q = np.arange(128)[None, :].astype(np.float64)
    for h in range(12):
        d = DIL[h // 4]
        prev = np.where(k >= q, np.exp(-slopes[h] * d * (q + 128 - k)), 0.0)
        cur = np.where(k <= q, np.exp(-slopes[h] * d * (q - k)), 0.0)
        alibi[h, :, 0:128] = prev
        alibi[h, :, 128:256] = cur
    rowm = (np.arange(128)[:, None] // 32 == np.arange(4)[None, :]).astype(np.float32)
    return ident, tri, seg, alibi, rowm


def make_in_maps(inputs, cores, ned=NE):
    f = lambda a: np.ascontiguousarray(np.asarray(a, dtype=np.float32))
    x = f(inputs["x"])
    c = f(inputs["c"])
    ident, tri, seg, alibi, rowm = _consts()
    norms = np.stack([f(inputs["pre_norm_mix"])[0], f(inputs["post_norm_mix"])[0],
                      f(inputs["pre_norm_ffn"])[0], f(inputs["post_norm_ffn"])[0]], 0)
    lbl = f(inputs["hgrn_lb_logits"]).reshape(2, 8, 128).transpose(2, 0, 1).reshape(128, 16)
    hn = f(inputs["hgrn_norm"])[0].reshape(8, 128).T
    def kmaj(w, cw):
        K, C = w.shape
        return f(np.asarray(w).reshape(K // 128, 128, C // cw, cw).transpose(2, 1, 0, 3))

    def kmaj_e(w, cw):
        E, K, C = w.shape
        return f(np.asarray(w).reshape(E, K // 128, 128, C // cw, cw).transpose(0, 3, 2, 1, 4))

    wde = np.asarray(inputs["w_down_e"][0, :ned])
    shared = {
        "w_ada": kmaj(inputs["w_ada"][0], 512), "b_ada": f(inputs["b_ada"]), "norms": f(norms), "w_in": kmaj(inputs["w_in"][0], 128),
        "lbl": f(lbl), "hn": f(hn), "w_ba": kmaj(inputs["w_branch_attn"][0], 128), "w_bh": kmaj(inputs["w_branch_hgrn"][0], 128),
        "w_out": kmaj(inputs["w_out"][0], 256), "w_r": kmaj(inputs["w_router"][0], NE)[0], "rb": f(inputs["router_bias"]),
        "w_ge": kmaj_e(inputs["w_gate_e"][0, :ned], 256), "w_ue": kmaj_e(inputs["w_up_e"][0, :ned], 256),
        "w_de": f(wde.reshape(wde.shape[0], 4, 128, D).transpose(0, 2, 1, 3)),
        "w_gs": kmaj(inputs["w_gate_s"][0], 256), "w_us": kmaj(inputs["w_up_s"][0], 256),
        "w_ds": f(np.asarray(inputs["w_down_s"][0]).reshape(4, 128, D).transpose(1, 0, 2)),
        "ident": ident, "tri32": tri, "segmask": seg, "alibi": alibi, "rowm": rowm,
    }
    maps = []
    for core in cores:
        b, half = core // 2, core % 2
        m = dict(shared)
        m["x_own"] = f(x[b, half * TOK:(half + 1) * TOK])
        m["x_halo"] = f(x[b, 0:TOK]) if half == 1 else np.zeros((TOK, D), np.float32)
        m["flag"] = np.full((128, 1), float(half), np.float32)
        m["cb"] = f(c[b].reshape(16, 128).T)
        maps.append(m)
    return maps


def kernel(**inputs):
    nc = build()
    cores = list(range(8))
    maps = make_in_maps(inputs, cores)
    res = run_bass_kernel_spmd(nc, maps, core_ids=cores)
    out = np.zeros((4, 4096, D), np.float32)
    for core in cores:
        b, half = core // 2, core % 2
        out[b, half * TOK:(half + 1) * TOK] = res.results[core]["out"]
    return out
```
